# Optimizing a Trainium2 kernel written in Bass

```python
import jax, jax.numpy as jnp
from jax import lax
import numpy as np

D_MODEL = 4096
BATCH = 2
SEQ = 8192
DEPTH = 1

N_HEADS = 16
N_KV_HEADS = 4
HEAD_DIM = 128
ATTN_WIDTH = N_HEADS * HEAD_DIM
KV_WIDTH = N_KV_HEADS * HEAD_DIM
IDX_HEADS = 16
IDX_DIM = 64
TOPK_MAX = 256
POOL_WINDOWS = (2, 4, 8, 16)
POOL_WIDTH = D_MODEL // 2
POOL_GROUP = POOL_WIDTH // len(POOL_WINDOWS)
MIX_WIDTH = ATTN_WIDTH + POOL_WIDTH
IN_SIZES = (ATTN_WIDTH, KV_WIDTH, KV_WIDTH, IDX_HEADS * IDX_DIM, IDX_DIM, IDX_HEADS, POOL_WIDTH)
IN_WIDTH = sum(IN_SIZES)
D_FF = -(-8 * D_MODEL // (3 * 256)) * 256
ROPE_THETA = 500000.0
ROT_FRACTION = 4
BLOCK_Q = 128
EPS = 1e-6

kernel_name = "hymba_style_dsa_pool_hybrid"


def rmsnorm(x, g):
    xf = x.astype(jnp.float32)
    y = xf * lax.rsqrt(jnp.mean(xf * xf, axis=-1, keepdims=True) + EPS)
    return (y * g.astype(jnp.float32)).astype(x.dtype)


def rope_tables(pos, rot_dim):
    inv_freq = ROPE_THETA ** (-jnp.arange(0, rot_dim, 2, dtype=jnp.float32) / rot_dim)
    ang = pos.astype(jnp.float32)[:, None] * inv_freq[None, :]
    return jnp.cos(ang), jnp.sin(ang)


def apply_partial_rope(x, cos, sin):
    rot = x.shape[-1] // ROT_FRACTION
    half = rot // 2
    xf = x.astype(jnp.float32)
    x1 = xf[..., :half]
    x2 = xf[..., half:rot]
    c = cos[:, None, :]
    s = sin[:, None, :]
    out = jnp.concatenate([x1 * c - x2 * s, x2 * c + x1 * s, xf[..., rot:]], axis=-1)
    return out.astype(x.dtype)


def dsa_sparse_attention(q, k, v, iq, ik, iw):
    B, S = q.shape[0], q.shape[1]
    topk = min(TOPK_MAX, S // 4)
    nb = S // BLOCK_Q
    group = N_HEADS // N_KV_HEADS
    scale = HEAD_DIM ** -0.5
    idx_scale = (IDX_DIM ** -0.5) * (IDX_HEADS ** -0.5)
    key_pos = jnp.arange(S)

    def to_blocks(a):
        return a.reshape((B, nb, BLOCK_Q) + a.shape[2:]).swapaxes(0, 1)

    qb, iqb, iwb = to_blocks(q), to_blocks(iq), to_blocks(iw)

    def block(args):
        blk, q_blk, iq_blk, iw_blk = args
        qpos = blk * BLOCK_Q + jnp.arange(BLOCK_Q)
        logits = jnp.einsum('bqhd,bsd->bqhs', iq_blk, ik).astype(jnp.float32)
        score = jnp.einsum('bqhs,bqh->bqs', jax.nn.relu(logits),
                           iw_blk.astype(jnp.float32)) * idx_scale
        causal = key_pos[None, :] <= qpos[:, None]
        score = jnp.where(causal[None], score, -jnp.inf)
        _, idx = lax.top_k(score, topk)
        valid = idx <= qpos[None, :, None]
        k_sel = jax.vmap(lambda kb, ib: kb[ib])(k, idx)
        v_sel = jax.vmap(lambda vb, ib: vb[ib])(v, idx)
        qg = q_blk.reshape(B, BLOCK_Q, N_KV_HEADS, group, HEAD_DIM)
        s = jnp.einsum('bqngd,bqknd->bqngk', qg, k_sel).astype(jnp.float32) * scale
        s = jnp.where(valid[:, :, None, None, :], s, -jnp.inf)
        p = jax.nn.softmax(s, axis=-1).astype(v.dtype)
        o = jnp.einsum('bqngk,bqknd->bqngd', p, v_sel)
        return o.reshape(B, BLOCK_Q, N_HEADS * HEAD_DIM)

    out = lax.map(block, (jnp.arange(nb), qb, iqb, iwb))
    return out.swapaxes(0, 1).reshape(B, S, N_HEADS * HEAD_DIM)


def multiscale_pool(u, w_pool, pool_scale):
    B, S, C = u.shape
    G = len(POOL_WINDOWS)
    uf = u.astype(jnp.float32)
    csum = jnp.cumsum(uf, axis=1)
    t = jnp.arange(S)
    pooled = []
    for g, w in enumerate(POOL_WINDOWS):
        cg = csum[..., g * POOL_GROUP:(g + 1) * POOL_GROUP]
        prev = jnp.pad(cg, ((0, 0), (w, 0), (0, 0)))[:, :S]
        cnt = jnp.minimum(t + 1, w).astype(jnp.float32)[None, :, None]
        pooled.append((cg - prev) / cnt)
    pooled = jnp.stack(pooled, axis=2)
    diff = (pooled - uf.reshape(B, S, G, POOL_GROUP)).astype(u.dtype)
    mixed = jnp.einsum('bsgc,gcd->bsgd', diff, w_pool)
    return mixed.reshape(B, S, C) * pool_scale


def setup_inputs(seed: int = 0) -> dict:
    key = jax.random.key(seed)
    ks = jax.random.split(key, 12)
    f32 = jnp.float32

    def dense(k, shape, fan_in):
        return jax.random.normal(k, shape, f32) * (fan_in ** -0.5)

    return {
        "x": jax.random.normal(ks[0], (BATCH, SEQ, D_MODEL), f32),
        "norm_mix_g": 1.0 + 0.02 * jax.random.normal(ks[1], (DEPTH, D_MODEL), f32),
        "w_in": dense(ks[2], (DEPTH, D_MODEL, IN_WIDTH), D_MODEL),
        "w_pool": dense(ks[3], (DEPTH, len(POOL_WINDOWS), POOL_GROUP, POOL_GROUP), POOL_GROUP),
        "pool_scale": 1.0 + 0.02 * jax.random.normal(ks[4], (DEPTH, POOL_WIDTH), f32),
        "w_out": dense(ks[5], (DEPTH, MIX_WIDTH, D_MODEL), MIX_WIDTH),
        "norm_ffn_g": 1.0 + 0.02 * jax.random.normal(ks[6], (DEPTH, D_MODEL), f32),
        "w_gate": dense(ks[7], (DEPTH, D_MODEL, D_FF), D_MODEL),
        "w_up": dense(ks[8], (DEPTH, D_MODEL, D_FF), D_MODEL),
        "w_down": dense(ks[9], (DEPTH, D_FF, D_MODEL), D_FF),
        "norm_final_g": 1.0 + 0.02 * jax.random.normal(ks[10], (D_MODEL,), f32),
    }


def reference(x, norm_mix_g, w_in, w_pool, pool_scale, w_out, norm_ffn_g,
              w_gate, w_up, w_down, norm_final_g):
    B, S, _ = x.shape
    pos = jnp.arange(S)
    cos_a, sin_a = rope_tables(pos, HEAD_DIM // ROT_FRACTION)
    cos_i, sin_i = rope_tables(pos, IDX_DIM // ROT_FRACTION)
    split_points = [int(v) for v in np.cumsum(IN_SIZES)[:-1]]
    for l in range(DEPTH):
        h = rmsnorm(x, norm_mix_g[l])
        z = jnp.einsum('bsd,de->bse', h, w_in[l])
        q, k, v, iq, ik, iw, u = jnp.split(z, split_points, axis=-1)
        q = apply_partial_rope(q.reshape(B, S, N_HEADS, HEAD_DIM), cos_a, sin_a)
        k = apply_partial_rope(k.reshape(B, S, N_KV_HEADS, HEAD_DIM), cos_a, sin_a)
        v = v.reshape(B, S, N_KV_HEADS, HEAD_DIM)
        iq = apply_partial_rope(iq.reshape(B, S, IDX_HEADS, IDX_DIM), cos_i, sin_i)
        ik = apply_partial_rope(ik.reshape(B, S, 1, IDX_DIM), cos_i, sin_i).reshape(B, S, IDX_DIM)
        attn = dsa_sparse_attention(q, k, v, iq, ik, iw)
        pool = multiscale_pool(u, w_pool[l], pool_scale[l])
        mixed = jnp.concatenate([attn, pool], axis=-1)
        x = x + jnp.einsum('bse,ed->bsd', mixed, w_out[l])
        h = rmsnorm(x, norm_ffn_g[l])
        a = jax.nn.silu(jnp.einsum('bsd,df->bsf', h, w_gate[l])) * jnp.einsum('bsd,df->bsf', h, w_up[l])
        x = x + jnp.einsum('bsf,fd->bsd', a, w_down[l])
    return rmsnorm(x, norm_final_g)
```

```python
import numpy as np
import ml_dtypes
import concourse.bass as bass
import concourse.mybir as mybir
from concourse.bass_utils import run_bass_kernel_spmd

F32 = mybir.dt.float32
BF16 = mybir.dt.bfloat16
AF = mybir.ActivationFunctionType
ALU = mybir.AluOpType
AX = mybir.AxisListType

ROPE_THETA = 500000.0
EPS = 1e-6
NBIS = 22
NEG = -1.0e30


class Cfg:
    def __init__(self, D=4096, S=8192, B=2):
        self.D, self.S, self.B = D, S, B
        self.DFF = -(-8 * D // (3 * 256)) * 256
        self.KC = D // 128
        self.FC = self.DFF // 128
        self.POOLW = D // 2
        self.PG = self.POOLW // 4
        self.PGC = self.PG // 128
        self.PC = self.POOLW // 128
        self.MIXC = 16 + self.PC
        self.NT = S // 512
        self.NOWN = S // 2048
        self.TOPK = min(256, S // 4)
        self.NE = 24 + self.PC
        self.INW = 2048 + 512 + 512 + 1024 + 64 + 16 + self.POOLW
        base, rem = divmod(self.FC, 4)
        self.QF = [base + (1 if i < rem else 0) for i in range(4)]
        self.QF = [q for q in self.QF if q > 0]


class T:
    def __init__(self, name, ap, lo=None, hi=None):
        self.name, self.ap, self.lo, self.hi = name, ap, lo, hi
        self.lastw = None
        self.rd = {}
        self.rd_dma = []
        self.ov = []
        self.chan = None

    def __getitem__(self, idx):
        return self.ap[idx]


class Op:
    __slots__ = ("eng", "fn", "deps", "sig", "val", "sem", "dma", "waits")

    def __init__(self, eng, fn, dma):
        self.eng, self.fn, self.dma = eng, fn, dma
        self.deps = []
        self.sig = False
        self.val = None
        self.sem = None


class Sched:
    ENGS = ("pe", "act", "dve", "pool", "sp")

    def __init__(self, nc, sems):
        self.nc = nc
        self.free_sems = list(sems)
        self.esem = {e: self.free_sems.pop() for e in self.ENGS}
        self.ecount = {e: 0 for e in self.ENGS}
        self.q = {e: [] for e in self.ENGS}
        self.known = {e: {} for e in self.ENGS}
        self.chans = {}
        self.tiles = []
        self.sb_tiles = []
        self.nblk = 0

    def chan(self, key):
        if key not in self.chans:
            self.chans[key] = [self.free_sems.pop(), 0]
        return self.chans[key]

    def reg_tile(self, t, sbuf=False):
        self.tiles.append(t)
        if sbuf:
            for o in self.sb_tiles:
                if o.lo < t.hi and t.lo < o.hi:
                    o.ov.append(t)
                    t.ov.append(o)
            self.sb_tiles.append(t)
        return t

    def op(self, eng, fn, reads=(), writes=(), dma=None):
        o = Op(eng, fn, dma)
        deps = []
        is_dma = dma is not None
        rset = []
        for t in reads:
            rset.append(t)
        wset = []
        for t in writes:
            wset.append(t)
            for a in t.ov:
                wset.append(a)
        for t in rset:
            for tt in [t] + t.ov:
                w = tt.lastw
                if w is not None:
                    deps.append(w)
        for t in wset:
            w = t.lastw
            if w is not None and (is_dma or w.dma is not None or w.eng != eng):
                if not (is_dma and w.dma == dma):
                    deps.append(w)
            for e2, r in t.rd.items():
                if is_dma or e2 != eng:
                    deps.append(r)
            deps.extend(t.rd_dma)
        o.deps = [d for d in deps if d is not o]
        for d in o.deps:
            d.sig = True
        if is_dma:
            ch = self.chan(dma)
            ch[1] += 1
            o.sem, o.val = ch[0], 16 * ch[1]
            o.sig = True
        for t in rset:
            if is_dma:
                t.rd_dma.append(o)
            else:
                t.rd[eng] = o
        for t in wset:
            t.lastw = o
            t.rd = {}
            t.rd_dma = []
        self.q[eng].append(o)
        return o

    def flush(self, final_wait_all=True):
        nc = self.nc
        for e in self.ENGS:
            for o in self.q[e]:
                if o.dma is None and o.sig:
                    self.ecount[e] += 1
                    o.sem, o.val = self.esem[e], self.ecount[e]
        for e in self.ENGS:
            kn = self.known[e]
            for o in self.q[e]:
                need = {}
                for d in o.deps:
                    if d.val is None:
                        continue
                    k = id(d.sem)
                    if k not in need or need[k][1] < d.val:
                        need[k] = (d.sem, d.val)
                ws = []
                for k, (s, v) in need.items():
                    if kn.get(k, 0) >= v:
                        continue
                    kn[k] = v
                    ws.append((s, v))
                o.waits = ws
        finals = []
        if final_wait_all:
            for key, (s, c) in self.chans.items():
                if c > 0:
                    finals.append((s, 16 * c))
            for e in self.ENGS:
                if self.ecount[e] > 0:
                    finals.append((self.esem[e], self.ecount[e]))
        qs = self.q
        esem = self.esem

        def replay(ename, eng, extra=()):
            for o in qs[ename]:
                for (s, v) in o.waits:
                    eng.wait_ge(s, v)
                ins = o.fn(eng)
                if o.dma is not None:
                    ins.then_inc(o.sem, 16)
                elif o.sig:
                    ins.then_inc(o.sem, 1)
            for (s, v) in extra:
                eng.wait_ge(s, v)

        with nc.Block() as block:
            @block.tensor
            def _(eng):
                replay("pe", eng)

            @block.scalar
            def _(eng):
                replay("act", eng)

            @block.vector
            def _(eng):
                replay("dve", eng)

            @block.gpsimd
            def _(eng):
                replay("pool", eng)

            @block.sync
            def _(eng):
                replay("sp", eng, finals)
        self.q = {e: [] for e in self.ENGS}
        for t in self.tiles:
            t.lastw = None
            t.rd = {}
            t.rd_dma = []


class Arena:
    def __init__(self, sched, handle, nbytes):
        self.s, self.h, self.n = sched, handle, nbytes
        self.top = 0

    def mark(self):
        return self.top

    def at(self, off):
        self.top = off

    def release(self, m):
        self.top = m

    def alloc(self, name, shape_free, dt):
        esz = 4 if dt == F32 else 2
        n = int(np.prod(shape_free))
        nb = (n * esz + 31) // 32 * 32
        lo = self.top
        assert lo + nb <= self.n, f"SBUF arena overflow allocating {name}: {lo}+{nb} > {self.n}"
        self.top = lo + nb
        ap = self.h[:, lo // 2:(lo + n * esz) // 2]
        if dt == F32:
            ap = ap.bitcast(F32)
        if len(shape_free) == 2:
            ap = ap.rearrange("p (a b) -> p a b", b=shape_free[1])
        elif len(shape_free) == 3:
            ap = ap.rearrange("p (a b c) -> p a b c", b=shape_free[1], c=shape_free[2])
        t = T(name, ap, lo, lo + nb)
        return self.s.reg_tile(t, sbuf=True)


class Ring:
    def __init__(self, tiles):
        self.tiles = tiles
        self.i = 0

    def next(self):
        t = self.tiles[self.i % len(self.tiles)]
        self.i += 1
        return t


def build_program(cfg):
    c = cfg
    KC, S, D = c.KC, c.S, c.D
    nc = bass.Bass("TRN2", target_bir_lowering=False)

    def din(name, shape, dt=F32):
        return nc.dram_tensor(name, list(shape), dt, kind="ExternalInput")

    d_xa = din("xa", [D, S])
    d_xo = din("xo", [D, c.NOWN * 576])
    d_gmix = din("gmix", [128, KC])
    d_gffn = din("gffn", [128, KC])
    d_gfin = din("gfin", [128, KC])
    d_wk = din("wk", [128, KC, 512])
    d_wv = din("wv", [128, KC, 512])
    d_wik = din("wik", [128, KC, 128])
    d_wiw = din("wiw", [128, KC, 16])
    d_win = din("win", [c.NE, 128, KC, 128])
    d_wpool = din("wpool", [128, 4 * c.PGC * c.PG])
    d_pscale = din("pscale", [128, c.PC])
    d_wout = din("wout", [KC, 128, c.MIXC, 128])
    d_wgate = din("wgate", [c.FC, 128, KC, 128])
    d_wup = din("wup", [c.FC, 128, KC, 128])
    d_wdown = din("wdown", [KC, 128, c.FC, 128])
    d_ropeA = din("ropeA", [4, 128, S])
    d_ropeB = din("ropeB", [4, 128, c.NOWN * 512])
    d_invcnt = din("invcnt", [128, c.NOWN * 4 * 512])
    d_cbias = din("cbias", [128, 512])
    d_cbf = din("cbf", [128, 512], BF16)
    d_cf32 = din("cf32", [128, 64])
    d_out = nc.dram_tensor("out", [D, c.NOWN * 512], F32, kind="ExternalOutput")
    d_dbg = nc.dram_tensor("dbg", [128, 16384], F32, kind="ExternalOutput") if getattr(c, "debug", False) else None
    d_kc = nc.dram_tensor("kcache", [4, 128, S], BF16, kind="Internal")
    d_vc = nc.dram_tensor("vcache", [S, 512], BF16, kind="Internal")
    d_ikc = nc.dram_tensor("ikcache", [128, S], BF16, kind="Internal")

    ARENA_BYTES = 196 * 1024
    import contextlib
    with contextlib.ExitStack() as es:
        arena_h = es.enter_context(nc.sbuf_tensor("arena", [128, ARENA_BYTES // 2], BF16))
        psum = []
        for i in range(8):
            psum.append(es.enter_context(nc.psum_tensor(f"ps{i}", [128, 512], F32)))
        sems = [es.enter_context(nc.semaphore(f"s{i}")) for i in range(96)]
        sch = Sched(nc, sems)
        ar = Arena(sch, arena_h, ARENA_BYTES)
        P = [sch.reg_tile(T(f"P{i}", psum[i][:])) for i in range(8)]
        P6b = psum[6].bitcast(BF16)
        T_kc = sch.reg_tile(T("kcache", None))
        T_vc = sch.reg_tile(T("vcache", None))
        T_ikc = sch.reg_tile(T("ikcache", None))
        T_out = sch.reg_tile(T("out", None))

        cbf = ar.alloc("cbf", (512,), BF16)
        cf32 = ar.alloc("cf32", (64,), F32)
        gmix = ar.alloc("gmix", (KC,), F32)
        gffn = ar.alloc("gffn", (KC,), F32)
        gfin = ar.alloc("gfin", (KC,), F32)
        ident = cbf.ap[:, 0:128]
        PaT = cbf.ap[:, 128:256]
        PiT = cbf.ap[:, 256:384]
        ones_bf = cbf.ap[:, 384:512]
        eps_ap = cf32.ap[:, 0:1]
        one_f = cf32.ap[:, 1:2]
        pow2 = cf32.ap[:, 8:8 + NBIS + 1]

        def load(eng, t, dst_ap, src_ap, chan, reads=()):
            sch.op(eng, lambda e, o=dst_ap, i=src_ap: e.dma_start(out=o, in_=i), reads=reads, writes=[t], dma=chan)

        load("sp", cbf, cbf.ap, d_cbf.ap(), "c_cbf")
        load("sp", cf32, cf32.ap, d_cf32.ap(), "c_cf32")
        load("sp", gmix, gmix.ap, d_gmix.ap(), "c_gmix")
        load("sp", gffn, gffn.ap, d_gffn.ap(), "c_gffn")
        load("sp", gfin, gfin.ap, d_gfin.ap(), "c_gfin")
        base_mark = ar.mark()

        T_dbg = sch.reg_tile(T("dbg", None))

        def dbg_dump(t, ap, col, n):
            if d_dbg is None:
                return
            sch.op("pool", lambda e: e.dma_start(out=d_dbg.ap()[:, col:col + n], in_=ap), reads=[t], writes=[T_dbg], dma="ch_dbg")

        def mm(out_t, out_ap, lhsT, rhs, start, stop, reads):
            sch.op("pe", lambda e: e.matmul(out_ap, lhsT, rhs, start=start, stop=stop), reads=reads, writes=[out_t])

        def rope(zf_t, zf_ap, PT, Ct, C_ap, St, S_ap, out_t, out_ap, tmp1, tmp2, n):
            sch.op("pe", lambda e: e.matmul(P[3].ap[:, 0:n], PT, zf_ap, start=True, stop=True),
                   reads=[zf_t, cbf], writes=[P[3]])
            sch.op("dve", lambda e: e.tensor_tensor(tmp1.ap[:, 0:n], zf_ap, C_ap, ALU.mult),
                   reads=[zf_t, Ct], writes=[tmp1])
            sch.op("dve", lambda e: e.tensor_tensor(tmp2.ap[:, 0:n], P[3].ap[:, 0:n], S_ap, ALU.mult),
                   reads=[P[3], St], writes=[tmp2])
            sch.op("dve", lambda e: e.tensor_tensor(out_ap, tmp1.ap[:, 0:n], tmp2.ap[:, 0:n], ALU.add),
                   reads=[tmp1, tmp2], writes=[out_t])

        def rstd_from(ps_t, ps_ap, tmp_t, tmp_ap, out_t, out_ap):
            sch.op("act", lambda e: e.activation(tmp_ap, ps_ap, AF.Sqrt, bias=eps_ap, scale=1.0 / D),
                   reads=[ps_t, cf32], writes=[tmp_t])
            sch.op("dve", lambda e: e.reciprocal(out_ap, tmp_ap), reads=[tmp_t], writes=[out_t])

        assert base_mark <= 12 * 1024
        ar.at(12 * 1024)
        wk = ar.alloc("wk", (KC, 512), BF16)
        wv = ar.alloc("wv", (KC, 512), BF16)
        wik = ar.alloc("wik", (KC, 128), BF16)
        load("pool", wk, wk.ap, d_wk.ap(), "c_wk")
        load("pool", wv, wv.ap, d_wv.ap(), "c_wv")
        load("pool", wik, wik.ap, d_wik.ap(), "c_wik")
        XG = 4
        NG = KC // XG
        xg_ring = Ring([ar.alloc(f"xg{i}", (XG, 512), F32) for i in range(2)])
        sq_ring = Ring([ar.alloc(f"sq{i}", (512,), BF16) for i in range(3)])
        hT = [[ar.alloc(f"hT{b}_{k}", (512,), BF16) for k in range(KC)] for b in range(2)]
        ropeA_t = ar.alloc("ropeA", (4, 512), F32)
        rsA = [ar.alloc(f"rsA{b}", (512,), F32) for b in range(2)]
        rsq = ar.alloc("rsq", (512,), F32)
        rtok = [ar.alloc(f"rtok{b}", (4,), F32) for b in range(2)]
        zf_ring = Ring([ar.alloc(f"zf{i}", (512,), BF16) for i in range(2)])
        tmp1 = ar.alloc("tmp1", (512,), F32)
        tmp2 = ar.alloc("tmp2", (512,), F32)
        ko_ring = Ring([ar.alloc(f"ko{i}", (512,), BF16) for i in range(3)])
        vo_ring = Ring([ar.alloc(f"vo{i}", (512,), BF16) for i in range(3)])
        pA_ring = Ring([P[1], P[2], P[4], P[5]])
        ssumA = [P[0], P[7]]

        def xstream_groups_A(t):
            b = t % 2
            groups = []
            for g in range(NG):
                def emit(g=g):
                    xg = xg_ring.next()
                    sch.op("sp", lambda e: e.dma_start(
                        out=xg.ap, in_=d_xa.ap()[g * XG * 128:(g + 1) * XG * 128, t * 512:(t + 1) * 512]
                        .rearrange("(k p) n -> p k n", p=128)), writes=[xg], dma="ch_" + xg.name)
                    for kk in range(XG):
                        kc = g * XG + kk
                        sq = sq_ring.next()
                        sch.op("act", lambda e, kk=kk, sq=sq: e.activation(sq.ap, xg.ap[:, kk, :], AF.Square),
                               reads=[xg], writes=[sq])
                        h = hT[b][kc]
                        sch.op("dve", lambda e, kk=kk, h=h, kc=kc: e.tensor_scalar(
                            h.ap, xg.ap[:, kk, :], gmix.ap[:, kc:kc + 1], None, ALU.mult),
                            reads=[xg, gmix], writes=[h])
                        mm(ssumA[b], ssumA[b].ap, ones_bf, sq.ap, kc == 0, kc == KC - 1, [sq, cbf])
                groups.append(emit)
            return groups

        def proj_groups_A(t):
            b = t % 2
            groups = []

            def g_rstd():
                sch.op("sp", lambda e: e.dma_start(
                    out=ropeA_t.ap, in_=d_ropeA.ap()[:, :, t * 512:(t + 1) * 512].rearrange("f p n -> p f n")),
                    writes=[ropeA_t], dma="ch_ropeA")
                rstd_from(ssumA[b], ssumA[b].ap, rsq, rsq.ap, rsA[b], rsA[b].ap)
                for tb in range(4):
                    sch.op("pe", lambda e, tb=tb: e.matmul(P[6].ap[:, tb:tb + 1], rsA[b].ap[0:1, tb * 128:(tb + 1) * 128],
                                                          one_f[0:1, :], start=True, stop=True),
                           reads=[rsA[b], cf32], writes=[P[6]])
                sch.op("act", lambda e: e.activation(rtok[b].ap, P[6].ap[:, 0:4], AF.Copy),
                       reads=[P[6]], writes=[rtok[b]])
            groups.append(g_rstd)

            def g_feat(ci):
                def emit():
                    ps = pA_ring.next()
                    for kc in range(KC):
                        lhsT = wk.ap[:, kc, ci * 128:(ci + 1) * 128] if ci < 4 else wik.ap[:, kc, :]
                        mm(ps, ps.ap, lhsT, hT[b][kc].ap, kc == 0, kc == KC - 1, [hT[b][kc], wk if ci < 4 else wik])
                    zf = zf_ring.next()
                    sch.op("dve", lambda e: e.tensor_tensor(zf.ap, ps.ap, rsA[b].ap, ALU.mult),
                           reads=[ps, rsA[b]], writes=[zf])
                    ko = ko_ring.next()
                    ti = 0 if ci < 4 else 2
                    rope(zf, zf.ap, PaT if ci < 4 else PiT, ropeA_t, ropeA_t.ap[:, ti, :], ropeA_t,
                         ropeA_t.ap[:, ti + 1, :], ko, ko.ap, tmp1, tmp2, 512)
                    if ci < 4:
                        sch.op("sp", lambda e: e.dma_start(out=d_kc.ap()[ci, :, t * 512:(t + 1) * 512], in_=ko.ap),
                               reads=[ko], writes=[T_kc], dma="ch_" + ko.name)
                    else:
                        sch.op("sp", lambda e: e.dma_start(out=d_ikc.ap()[:, t * 512:(t + 1) * 512], in_=ko.ap),
                               reads=[ko], writes=[T_ikc], dma="ch_" + ko.name)
                return emit
            for ci in range(5):
                groups.append(g_feat(ci))

            def g_v(tb):
                def emit():
                    ps = pA_ring.next()
                    for kc in range(KC):
                        mm(ps, ps.ap, hT[b][kc].ap[:, tb * 128:(tb + 1) * 128], wv.ap[:, kc, :], kc == 0, kc == KC - 1,
                           [hT[b][kc], wv])
                    vo = vo_ring.next()
                    sch.op("act", lambda e: e.activation(vo.ap, ps.ap, AF.Copy, scale=rtok[b].ap[:, tb:tb + 1]),
                           reads=[ps, rtok[b]], writes=[vo])
                    sch.op("sp", lambda e: e.dma_start(
                        out=d_vc.ap()[t * 512 + tb * 128: t * 512 + (tb + 1) * 128, :], in_=vo.ap),
                        reads=[vo], writes=[T_vc], dma="ch_" + vo.name)
                return emit
            for tb in range(4):
                groups.append(g_v(tb))
            return groups

        for g in xstream_groups_A(0):
            g()
        for t in range(c.NT):
            pg = proj_groups_A(t)
            xg = xstream_groups_A(t + 1) if t + 1 < c.NT else []
            n = max(len(pg), len(xg))
            for i in range(n):
                if i < len(pg):
                    pg[i]()
                if i < len(xg):
                    xg[i]()
        sch.flush()
        ar.release(base_mark)

        NQ = 576
        KB = 1024
        ar.at(base_mark)
        wiw = ar.alloc("wiw", (KC, 16), BF16)
        pscale = ar.alloc("pscale", (c.PC,), F32)
        cbias = ar.alloc("cbias", (512,), F32)
        iw_sb = ar.alloc("iw_sb", (4, 16), F32)
        rs2 = ar.alloc("rs2", (512,), F32)
        assert ar.top <= 12 * KB, ar.top
        load("pool", wiw, wiw.ap, d_wiw.ap(), "c_wiw")
        load("sp", pscale, pscale.ap, d_pscale.ap(), "c_pscale")
        load("sp", cbias, cbias.ap, d_cbias.ap(), "c_cbias")
        WS = max(KC, c.MIXC, max(c.QF))
        ar.at(12 * KB)
        wring = Ring([ar.alloc(f"wr{i}", (WS, 128), BF16) for i in range(4)])
        mixT = ar.alloc("mixT", (c.MIXC, 512), BF16)
        OFF_H = ar.top
        hTB = [ar.alloc(f"hTB{k}", (NQ,), BF16) for k in range(KC)]
        OFF_Q = ar.top
        qT = ar.alloc("qT", (16, 512), BF16)
        iqT = ar.alloc("iqT", (8, 512), BF16)
        OFF_R = ar.top
        rsB = ar.alloc("rsB", (NQ,), F32)
        rsqB = ar.alloc("rsqB", (NQ,), F32)
        OFF_R2 = ar.top
        OFF_MIX = mixT.lo

        wstream = []

        def ws_add(ap, nk):
            wstream.append((ap, nk))
        for i in range(c.NOWN):
            for e_ in range(c.NE):
                ws_add(d_win.ap()[e_], KC)
            for dc in range(KC):
                ws_add(d_wout.ap()[dc], c.MIXC)
            f0 = 0
            for qn in c.QF:
                for f in range(f0, f0 + qn):
                    ws_add(d_wgate.ap()[f], KC)
                    ws_add(d_wup.ap()[f], KC)
                for dc in range(KC):
                    ws_add(d_wdown.ap()[dc, :, f0:f0 + qn, :], qn)
                f0 += qn
        ws_state = {"issued": 0, "consumed": 0}
        ws_slots = {}

        def ws_issue_upto(n):
            while ws_state["issued"] < min(n, len(wstream)):
                k = ws_state["issued"]
                ap, nk = wstream[k]
                slot = wring.next()
                ws_slots[k] = slot
                sch.op("pool", lambda e, slot=slot, ap=ap, nk=nk: e.dma_start(out=slot.ap[:, 0:nk, :], in_=ap),
                       writes=[slot], dma="ch_" + slot.name)
                ws_state["issued"] += 1

        def ws_get():
            k = ws_state["consumed"]
            ws_issue_upto(k + 1)
            ws_state["consumed"] += 1
            return ws_slots[k], k

        def ws_prefetch():
            ws_issue_upto(ws_state["consumed"] + 4)

        pB_ring = Ring([P[1], P[2], P[4], P[5]])

        for i in range(c.NOWN):
            ar.at(OFF_R2)
            XGB = 2
            NGB = KC // XGB
            xgB = Ring([ar.alloc(f"xgB{u}", (XGB, NQ), F32) for u in range(2)])
            sqB = Ring([ar.alloc(f"sqB{u}", (NQ,), BF16) for u in range(3)])
            tabm = ar.mark()
            ropeB_t = ar.alloc("ropeB", (4, 512), F32)
            ar.at(tabm)
            invc = ar.alloc("invc", (4, 512), F32)
            wpool = Ring([ar.alloc(f"wpool{u}", (c.PGC, c.PG), BF16) for u in range(2)])
            zfB = Ring([ar.alloc(f"zfB{u}", (512,), BF16) for u in range(2)])
            t1B = ar.alloc("t1B", (NQ,), F32)
            t2B = ar.alloc("t2B", (NQ,), F32)
            uB = Ring([ar.alloc(f"uB{u}", (NQ,), F32) for u in range(2)])
            sA = ar.alloc("sA", (NQ,), F32)
            sBt = ar.alloc("sBt", (NQ,), F32)
            diffR = Ring([[ar.alloc(f"diffT{u}_{v}", (512,), BF16) for v in range(c.PGC)] for u in range(2)])
            load("sp", ropeB_t, ropeB_t.ap, d_ropeB.ap()[:, :, i * 512:(i + 1) * 512].rearrange("f p n -> p f n"), "ch_ropeB")
            halves = [(0, NQ // 2), (NQ // 2, NQ)]
            for g in range(NGB):
                xg = xgB.next()
                sch.op("sp", lambda e, g=g, xg=xg, i=i: e.dma_start(
                    out=xg.ap, in_=d_xo.ap()[g * XGB * 128:(g + 1) * XGB * 128, i * NQ:(i + 1) * NQ]
                    .rearrange("(k p) n -> p k n", p=128)), writes=[xg], dma="ch_" + xg.name)
                for kk in range(XGB):
                    kc = g * XGB + kk
                    sq = sqB.next()
                    sch.op("act", lambda e, kk=kk, sq=sq, xg=xg: e.activation(sq.ap, xg.ap[:, kk, :], AF.Square),
                           reads=[xg], writes=[sq])
                    h = hTB[kc]
                    sch.op("dve", lambda e, kk=kk, h=h, kc=kc, xg=xg: e.tensor_scalar(
                        h.ap, xg.ap[:, kk, :], gmix.ap[:, kc:kc + 1], None, ALU.mult),
                        reads=[xg, gmix], writes=[h])
                    for hi_, (a, b_) in enumerate(halves):
                        pt = (P[0], P[7])[hi_]
                        mm(pt, pt.ap[:, 0:b_ - a], ones_bf, sq.ap[:, a:b_], kc == 0, kc == KC - 1, [sq, cbf])
            for hi_, (a, b_) in enumerate(halves):
                pt = (P[0], P[7])[hi_]
                rstd_from(pt, pt.ap[:, 0:b_ - a], rsqB, rsqB.ap[:, a:b_], rsB, rsB.ap[:, a:b_])

            def main_cols(ap2d):
                return ap2d.rearrange("p (r c) -> p r c", c=144)[:, :, 16:144]

            rsB_main = main_cols(rsB.ap)
            for e_ in range(c.NE):
                slot, _k = ws_get()
                if e_ < 24:
                    ps = pB_ring.next()
                    for kc in range(KC):
                        mm(ps, ps.ap.rearrange("p (r c) -> p r c", c=128), slot.ap[:, kc, :], main_cols(hTB[kc].ap),
                           kc == 0, kc == KC - 1, [hTB[kc], slot])
                    ws_prefetch()
                    zf = zfB.next()
                    sch.op("dve", lambda e, ps=ps, zf=zf: e.tensor_tensor(
                        zf.ap.rearrange("p (r c) -> p r c", c=128), ps.ap.rearrange("p (r c) -> p r c", c=128),
                        rsB_main, ALU.mult), reads=[ps, rsB], writes=[zf])
                    if e_ < 16:
                        rope(zf, zf.ap, PaT, ropeB_t, ropeB_t.ap[:, 0, :], ropeB_t, ropeB_t.ap[:, 1, :],
                             qT, qT.ap[:, e_, :], t1B, t2B, 512)
                    else:
                        rope(zf, zf.ap, PiT, ropeB_t, ropeB_t.ap[:, 2, :], ropeB_t, ropeB_t.ap[:, 3, :],
                             iqT, iqT.ap[:, e_ - 16, :], t1B, t2B, 512)
                else:
                    uc = e_ - 24
                    g_ = uc // c.PGC
                    kcl = uc % c.PGC
                    w = (2, 4, 8, 16)[g_]
                    if uc == 0:
                        load("sp", invc, invc.ap, d_invcnt.ap()[:, i * 2048:(i + 1) * 2048]
                             .rearrange("p (g n) -> p g n", g=4), "ch_invc")
                    if kcl == 0:
                        wpl = wpool.next()
                        diffT = diffR.next()
                        load("pool", wpl, wpl.ap, d_wpool.ap()[:, g_ * c.PGC * c.PG:(g_ + 1) * c.PGC * c.PG]
                             .rearrange("p (a b) -> p a b", b=c.PG), "ch_" + wpl.name)
                    pss = [pB_ring.next(), pB_ring.next()]
                    for kc in range(KC):
                        for hi_, (a, b_) in enumerate(halves):
                            mm(pss[hi_], pss[hi_].ap[:, 0:b_ - a], slot.ap[:, kc, :], hTB[kc].ap[:, a:b_],
                               kc == 0, kc == KC - 1, [hTB[kc], slot])
                    ws_prefetch()
                    u = uB.next()
                    for hi_, (a, b_) in enumerate(halves):
                        sch.op("dve", lambda e, hi_=hi_, a=a, b_=b_, u=u, pss=pss: e.tensor_tensor(
                            u.ap[:, a:b_], pss[hi_].ap[:, 0:b_ - a], rsB.ap[:, a:b_], ALU.mult),
                            reads=[pss[hi_], rsB], writes=[u])
                    cur, cur_t = u.ap, u
                    sh = 1
                    bufs = [sA, sBt]
                    bi = 0
                    while sh < w:
                        nt_ = bufs[bi % 2]
                        bi += 1
                        sch.op("dve", lambda e, cur=cur, nt_=nt_, sh=sh: e.tensor_tensor(
                            nt_.ap[:, sh:NQ], cur[:, sh:NQ], cur[:, 0:NQ - sh], ALU.add),
                            reads=[cur_t], writes=[nt_])
                        cur, cur_t = nt_.ap, nt_
                        sh *= 2
                    sch.op("dve", lambda e, cur=cur, g_=g_: e.tensor_tensor(
                        t1B.ap.rearrange("p (r c) -> p r c", c=144)[:, :, 0:128], main_cols(cur),
                        invc.ap[:, g_, :].rearrange("p (r c) -> p r c", c=128), ALU.mult),
                        reads=[cur_t, invc], writes=[t1B])
                    dt_ = diffT[kcl]
                    sch.op("dve", lambda e, u=u, dt_=dt_: e.tensor_tensor(
                        dt_.ap.rearrange("p (r c) -> p r c", c=128),
                        t1B.ap.rearrange("p (r c) -> p r c", c=144)[:, :, 0:128], main_cols(u.ap), ALU.subtract),
                        reads=[t1B, u], writes=[dt_])
                    if kcl == c.PGC - 1:
                        for dcn in range(c.PGC):
                            ps = pB_ring.next()
                            for kc in range(c.PGC):
                                mm(ps, ps.ap, wpl.ap[:, kc, dcn * 128:(dcn + 1) * 128], diffT[kc].ap,
                                   kc == 0, kc == c.PGC - 1, [diffT[kc], wpl])
                            ch = g_ * c.PGC + dcn
                            sch.op("act", lambda e, ps=ps, ch=ch: e.activation(mixT.ap[:, 16 + ch, :], ps.ap, AF.Copy,
                                                                             scale=pscale.ap[:, ch:ch + 1]),
                                   reads=[ps, pscale], writes=[mixT])
            for r in range(4):
                for kc in range(KC):
                    mm(P[6], P[6].ap[:, 16 * r:16 * r + 16], hTB[kc].ap[:, r * 144 + 16:r * 144 + 144], wiw.ap[:, kc, :],
                       kc == 0, kc == KC - 1, [hTB[kc], wiw])
            sch.op("act", lambda e: e.activation(iw_sb.ap.rearrange("p a b -> p (a b)"), P[6].ap[:, 0:64], AF.Copy),
                   reads=[P[6]], writes=[iw_sb])

            ar.at(OFF_H)
            scores = ar.alloc("scores", (S,), F32)
            diag_in_h = (OFF_Q - ar.top) >= 16 * 128 * 2
            if diag_in_h:
                diag = ar.alloc("diag", (16, 128), BF16)
            assert ar.top <= OFF_Q
            ar.at(OFF_R2)
            if not diag_in_h:
                diag = ar.alloc("diag", (16, 128), BF16)
            maskq = ar.alloc("maskq", (S,), BF16)
            maskT = [ar.alloc("maskT0", (S // 128, 128), BF16)] * 2
            ik_ring = Ring([ar.alloc(f"ikr{u}", (512,), BF16) for u in range(3)])
            R_ring = Ring([ar.alloc(f"Rr{u}", (512,), BF16) for u in range(3)])
            k_ring = Ring([ar.alloc(f"kr{u}", (512,), BF16) for u in range(3)])
            v_ring = Ring([ar.alloc(f"vr{u}", (4, 128), BF16) for u in range(3)])
            E_ring = Ring([ar.alloc(f"Er{u}", (512,), BF16) for u in range(3)])
            Pm_ring = Ring([ar.alloc(f"Pm{u}", (512,), BF16) for u in range(3)])
            rden = ar.alloc("rden", (512,), F32)
            bis = ar.alloc("bis", (4 * (NBIS + 4),), F32)
            L_ring = Ring([P[0], P[7]])
            ST_ring = Ring([P[1], P[2]])

            def indexer(r):
                m = 4 * i + r
                ns = m + 1
                n = 512 * ns
                for h in range(16):
                    sch.op("dve", lambda e, h=h: e.tensor_scalar(diag.ap[:, h, :], ident, iw_sb.ap[:, r, h:h + 1], None, ALU.mult),
                           reads=[cbf, iw_sb], writes=[diag])
                for s in range(ns):
                    ikt = ik_ring.next()
                    sch.op("sp", lambda e, s=s, ikt=ikt: e.dma_start(out=ikt.ap, in_=d_ikc.ap()[:, s * 512:(s + 1) * 512]),
                           reads=[T_ikc], writes=[ikt], dma="ch_" + ikt.name)
                    pend = []
                    for h in range(17):
                        if h < 16:
                            cch, off = h // 2, 64 * (h % 2)
                            L = L_ring.next()
                            mm(L, L.ap, iqT.ap[off:off + 64, cch, r * 128:(r + 1) * 128], ikt.ap[off:off + 64, :],
                               True, True, [iqT, ikt])
                            Rt = R_ring.next()
                            sch.op("act", lambda e, L=L, Rt=Rt: e.activation(Rt.ap, L.ap, AF.Relu), reads=[L], writes=[Rt])
                            pend.append((h, Rt))
                        if h >= 1:
                            hh, Rt2 = pend.pop(0)
                            mm(P[3], P[3].ap, diag.ap[:, hh, :], Rt2.ap, hh == 0, hh == 15, [diag, Rt2])
                    sch.op("act", lambda e, s=s: e.activation(scores.ap[:, s * 512:(s + 1) * 512], P[3].ap, AF.Copy),
                           reads=[P[3]], writes=[scores])
                lo0 = bis.ap[:, 0:1]
                hi0 = bis.ap[:, 1:2]
                rng = bis.ap[:, 2:3]
                steps = bis.ap[:, 4:4 + NBIS + 1]
                sch.op("dve", lambda e: e.tensor_reduce(hi0, scores.ap[:, 0:n], AX.X, ALU.max), reads=[scores], writes=[bis])
                sch.op("dve", lambda e: e.tensor_reduce(lo0, scores.ap[:, 0:n], AX.X, ALU.min), reads=[scores], writes=[bis])
                sch.op("dve", lambda e: e.tensor_tensor(scores.ap[:, n - 512:n], scores.ap[:, n - 512:n], cbias.ap, ALU.add),
                       reads=[scores, cbias], writes=[scores])
                sch.op("dve", lambda e: e.tensor_tensor(rng, hi0, lo0, ALU.subtract), reads=[bis], writes=[bis])
                sch.op("dve", lambda e: e.tensor_scalar(steps, pow2, rng, None, ALU.mult), reads=[bis, cf32], writes=[bis])
                base = 4 + NBIS + 1
                lo_c = [bis.ap[:, base + 0:base + 1], bis.ap[:, base + 1:base + 2]]
                t_c = [bis.ap[:, base + 2:base + 3], bis.ap[:, base + 3:base + 4]]
                cnt = bis.ap[:, base + 4:base + 5]
                Aap = bis.ap[:, base + 5:base + 6]
                sch.op("dve", lambda e: e.tensor_copy(lo_c[0], lo0), reads=[bis], writes=[bis])
                sch.op("dve", lambda e: e.tensor_tensor(t_c[0], lo0, steps[:, 0:1], ALU.add), reads=[bis], writes=[bis])
                for k in range(NBIS):
                    a, b_ = k % 2, (k + 1) % 2
                    sch.op("dve", lambda e, a=a: e.tensor_scalar(maskq.ap[:, 0:n], scores.ap[:, 0:n], t_c[a], 0.0,
                                                                 ALU.is_ge, ALU.add, accum_out=cnt),
                           reads=[scores, bis], writes=[maskq, bis])
                    sch.op("dve", lambda e, k=k: e.tensor_scalar(Aap, cnt, float(c.TOPK) - 0.5, steps[:, k:k + 1],
                                                                 ALU.is_ge, ALU.mult), reads=[bis], writes=[bis])
                    sch.op("dve", lambda e, a=a, b_=b_, k=k: e.scalar_tensor_tensor(
                        t_c[b_], Aap, steps[:, k + 1:k + 2], lo_c[a], ALU.add, ALU.add), reads=[bis], writes=[bis])
                    sch.op("dve", lambda e, a=a, b_=b_: e.tensor_tensor(lo_c[b_], Aap, lo_c[a], ALU.add),
                           reads=[bis], writes=[bis])
                lof = lo_c[NBIS % 2]
                sch.op("dve", lambda e: e.tensor_scalar(maskq.ap[:, 0:n], scores.ap[:, 0:n], lof, None, ALU.is_ge),
                       reads=[scores, bis], writes=[maskq])
                if i == 0 and r == 1:
                    dbg_dump(scores, scores.ap[:, 0:1024], 0, 1024)
                    dbg_dump(bis, bis.ap[:, 0:64], 1024, 64)
                    dbg_dump(maskq, maskq.ap[:, 0:1024], 2048, 1024)
                    dbg_dump(qT, qT.ap[:, 0, :], 4096, 512)
                    dbg_dump(iqT, iqT.ap[:, 0, :], 4608, 512)
                    dbg_dump(iw_sb, iw_sb.ap.rearrange("p a b -> p (a b)"), 5120, 64)

            def mask_transpose(r):
                m = 4 * i + r
                ns = m + 1
                mt = maskT[r % 2]
                for s in range(ns):
                    for j4 in range(4):
                        kc = 4 * s + j4
                        sch.op("pe", lambda e, kc=kc, j4=j4: e.transpose(P6b[:, j4 * 128:(j4 + 1) * 128],
                                                                        maskq.ap[:, kc * 128:(kc + 1) * 128], ident),
                               reads=[maskq, cbf], writes=[P[6]])
                    sch.op("act", lambda e, s=s, mt=mt: e.activation(mt.ap[:, 4 * s:4 * s + 4, :].rearrange("p a b -> p (a b)"),
                                                                     P6b[:, 0:512], AF.Copy), reads=[P[6]], writes=[mt])

            def attention(r):
                m = 4 * i + r
                ns = m + 1
                mt = maskT[r % 2]
                for nkv in range(4):
                    pend = []
                    for s in range(ns + 1):
                        if s < ns:
                            kt = k_ring.next()
                            vt = v_ring.next()
                            sch.op("sp", lambda e, s=s, kt=kt, nkv=nkv: e.dma_start(out=kt.ap, in_=d_kc.ap()[nkv, :, s * 512:(s + 1) * 512]),
                                   reads=[T_kc], writes=[kt], dma="ch_" + kt.name)
                            sch.op("sp", lambda e, s=s, vt=vt, nkv=nkv: e.dma_start(
                                out=vt.ap, in_=d_vc.ap()[s * 512:(s + 1) * 512, nkv * 128:(nkv + 1) * 128]
                                .rearrange("(a p) d -> p a d", p=128)), reads=[T_vc], writes=[vt], dma="ch_" + vt.name)
                        for j4 in range(4):
                            if s < ns:
                                kc = 4 * s + j4
                                ST = ST_ring.next()
                                mm(ST, ST.ap.rearrange("p (g q) -> p g q", q=128), kt.ap[:, j4 * 128:(j4 + 1) * 128],
                                   qT.ap[:, 4 * nkv:4 * nkv + 4, r * 128:(r + 1) * 128], True, True, [kt, qT])
                                Et = E_ring.next()
                                sch.op("act", lambda e, ST=ST, Et=Et: e.activation(Et.ap, ST.ap, AF.Exp, scale=128.0 ** -0.5),
                                       reads=[ST], writes=[Et])
                                Pm = Pm_ring.next()
                                sch.op("dve", lambda e, Et=Et, Pm=Pm, kc=kc: e.tensor_tensor(
                                    Pm.ap.rearrange("p (g q) -> p g q", q=128), Et.ap.rearrange("p (g q) -> p g q", q=128),
                                    mt.ap[:, kc:kc + 1, :].to_broadcast([128, 4, 128]), ALU.mult),
                                    reads=[Et, mt], writes=[Pm])
                                pend.append((kc, Pm, vt, j4))
                                if i == 0 and r == 1 and nkv == 0 and kc == 0:
                                    dbg_dump(kt, kt.ap, 7168, 512)
                                    dbg_dump(vt, vt.ap.rearrange("p a b -> p (a b)"), 7680, 512)
                                    dbg_dump(Et, Et.ap, 9216, 512)
                                    dbg_dump(Pm, Pm.ap, 9728, 512)
                                    dbg_dump(mt, mt.ap[:, 0:8, :].rearrange("p a b -> p (a b)"), 8192, 1024)
                            if len(pend) > (1 if s < ns else 0):
                                kc2, Pm2, vt2, j42 = pend.pop(0)
                                last = (kc2 == 4 * ns - 1)
                                mm(P[4], P[4].ap, vt2.ap[:, j42, :], Pm2.ap, kc2 == 0, last, [vt2, Pm2])
                                mm(P[5], P[5].ap, ones_bf, Pm2.ap, kc2 == 0, last, [cbf, Pm2])
                    while pend:
                        kc2, Pm2, vt2, j42 = pend.pop(0)
                        last = (kc2 == 4 * ns - 1)
                        mm(P[4], P[4].ap, vt2.ap[:, j42, :], Pm2.ap, kc2 == 0, last, [vt2, Pm2])
                        mm(P[5], P[5].ap, ones_bf, Pm2.ap, kc2 == 0, last, [cbf, Pm2])
                    sch.op("dve", lambda e: e.reciprocal(rden.ap, P[5].ap), reads=[P[5]], writes=[rden])
                    if i == 0 and r == 1 and nkv == 0:
                        dbg_dump(rden, rden.ap, 10240, 512)
                    sch.op("dve", lambda e, nkv=nkv: e.tensor_tensor(
                        mixT.ap[:, 4 * nkv:4 * nkv + 4, r * 128:(r + 1) * 128], P[4].ap.rearrange("p (g q) -> p g q", q=128),
                        rden.ap.rearrange("p (g q) -> p g q", q=128), ALU.mult), reads=[P[4], rden], writes=[mixT])

            indexer(0)
            mask_transpose(0)
            for r in range(4):
                if r + 1 < 4:
                    indexer(r + 1)
                attention(r)
                if r + 1 < 4:
                    mask_transpose(r + 1)
            if i == 0:
                dbg_dump(mixT, mixT.ap[:, 0, :], 6144, 512)

            ar.at(OFF_Q)
            x1T = [ar.alloc(f"x1T{g}", (XG, 512), F32) for g in range(NG)]
            sq2 = Ring([ar.alloc(f"sq2_{u}", (512,), BF16) for u in range(3)])
            sg_ring = Ring([ar.alloc(f"sg{u}", (512,), F32) for u in range(2)])
            rsq4 = ar.alloc("rsq4", (512,), F32)
            ar.at(OFF_MIX)
            aT = ar.alloc("aT", (max(c.QF), 512), BF16)
            assert aT.hi <= OFF_H
            h2T = hTB
            for g in range(NG):
                for kk in range(XG):
                    kc = g * XG + kk
                    sch.op("sp", lambda e, g=g, kk=kk, kc=kc, i=i: e.dma_start(
                        out=x1T[g].ap[:, kk, :].rearrange("p (r c) -> p r c", c=128),
                        in_=d_xo.ap()[kc * 128:(kc + 1) * 128, i * NQ:(i + 1) * NQ]
                        .rearrange("p (r c) -> p r c", c=144)[:, :, 16:144]),
                        writes=[x1T[g]], dma="ch_" + x1T[g].name)

            def x1(kc):
                return x1T[kc // XG], x1T[kc // XG].ap[:, kc % XG, :]

            def norm_accum(kc):
                xt_, xap = x1(kc)
                sq = sq2.next()
                sch.op("act", lambda e: e.activation(sq.ap, xap, AF.Square), reads=[xt_], writes=[sq])
                mm(P[0], P[0].ap, ones_bf, sq.ap, kc == 0, kc == KC - 1, [sq, cbf])

            for dc in range(KC):
                slot, _k = ws_get()
                ps = pB_ring.next()
                for kc in range(c.MIXC):
                    mm(ps, ps.ap, slot.ap[:, kc, :], mixT.ap[:, kc, :], kc == 0, kc == c.MIXC - 1, [mixT, slot])
                ws_prefetch()
                xt_, xap = x1(dc)
                sch.op("dve", lambda e, ps=ps, xap=xap: e.tensor_tensor(xap, ps.ap, xap, ALU.add), reads=[ps, xt_], writes=[xt_])
                norm_accum(dc)
            rstd_from(P[0], P[0].ap, rsq4, rsq4.ap, rs2, rs2.ap)
            for kc in range(KC):
                xt_, xap = x1(kc)
                sch.op("dve", lambda e, kc=kc, xap=xap: e.scalar_tensor_tensor(
                    h2T[kc].ap[:, 0:512], xap, gffn.ap[:, kc:kc + 1], rs2.ap, ALU.mult, ALU.mult),
                    reads=[xt_, gffn, rs2], writes=[h2T[kc]])
            f0 = 0
            for qi, qn in enumerate(c.QF):
                for fl in range(qn):
                    sg_, _k = ws_get()
                    su_, _k = ws_get()
                    pg_, pu_ = pB_ring.next(), pB_ring.next()
                    for kc in range(KC):
                        mm(pg_, pg_.ap, sg_.ap[:, kc, :], h2T[kc].ap[:, 0:512], kc == 0, kc == KC - 1, [h2T[kc], sg_])
                    for kc in range(KC):
                        mm(pu_, pu_.ap, su_.ap[:, kc, :], h2T[kc].ap[:, 0:512], kc == 0, kc == KC - 1, [h2T[kc], su_])
                    ws_prefetch()
                    sgt = sg_ring.next()
                    sch.op("act", lambda e, pg_=pg_, sgt=sgt: e.activation(sgt.ap, pg_.ap, AF.Silu), reads=[pg_], writes=[sgt])
                    sch.op("dve", lambda e, pu_=pu_, sgt=sgt, fl=fl: e.tensor_tensor(aT.ap[:, fl, :], pu_.ap, sgt.ap, ALU.mult),
                           reads=[pu_, sgt], writes=[aT])
                lastq = (qi == len(c.QF) - 1)
                for dc in range(KC):
                    slot, _k = ws_get()
                    ps = pB_ring.next()
                    for fl in range(qn):
                        mm(ps, ps.ap, slot.ap[:, fl, :], aT.ap[:, fl, :], fl == 0, fl == qn - 1, [aT, slot])
                    ws_prefetch()
                    xt_, xap = x1(dc)
                    sch.op("dve", lambda e, ps=ps, xap=xap: e.tensor_tensor(xap, ps.ap, xap, ALU.add),
                           reads=[ps, xt_], writes=[xt_])
                    if lastq:
                        norm_accum(dc)
                f0 += qn
            rstd_from(P[0], P[0].ap, rsq4, rsq4.ap, rs2, rs2.ap)
            for g in range(NG):
                for kk in range(XG):
                    kc = g * XG + kk
                    xt_, xap = x1(kc)
                    sch.op("dve", lambda e, kc=kc, xap=xap: e.scalar_tensor_tensor(
                        xap, xap, gfin.ap[:, kc:kc + 1], rs2.ap, ALU.mult, ALU.mult),
                        reads=[xt_, gfin, rs2], writes=[xt_])
                sch.op("sp", lambda e, g=g, i=i: e.dma_start(
                    out=d_out.ap()[g * XG * 128:(g + 1) * XG * 128, i * 512:(i + 1) * 512].rearrange("(k p) n -> p k n", p=128),
                    in_=x1T[g].ap), reads=[x1T[g]], writes=[T_out], dma="ch_" + x1T[g].name)
        sch.flush()
    return nc


def rope_table(pos, rot_dim, head_dim, nrep):
    half = rot_dim // 2
    inv_freq = (ROPE_THETA ** (-np.arange(0, rot_dim, 2, dtype=np.float32) / np.float32(rot_dim))).astype(np.float32)
    ang = pos.astype(np.float32)[None, :] * inv_freq[:, None]
    cos, sin = np.cos(ang).astype(np.float32), np.sin(ang).astype(np.float32)
    C = np.ones((head_dim, len(pos)), np.float32)
    Sg = np.zeros((head_dim, len(pos)), np.float32)
    C[0:half] = cos
    C[half:rot_dim] = cos
    Sg[0:half] = -sin
    Sg[half:rot_dim] = sin
    return np.tile(C, (nrep, 1)), np.tile(Sg, (nrep, 1))


def perm_T(rot_dim, head_dim, nrep):
    half = rot_dim // 2
    PT = np.zeros((128, 128), np.float32)
    for rep in range(nrep):
        o = rep * head_dim
        for m in range(half):
            PT[o + m + half, o + m] = 1.0
        for m in range(half, rot_dim):
            PT[o + m - half, o + m] = 1.0
    return PT


def slab(W, ncols_chunks=None):
    K, N = W.shape
    return np.ascontiguousarray(W.reshape(K // 128, 128, N // 128, 128).transpose(2, 1, 0, 3))


def pkc(W):
    K, N = W.shape
    return np.ascontiguousarray(W.reshape(K // 128, 128, N).transpose(1, 0, 2))


def vec_pk(v):
    return np.ascontiguousarray(v.reshape(-1, 128).T)


_PROG_CACHE = {}


def kernel(x, norm_mix_g, w_in, w_pool, pool_scale, w_out, norm_ffn_g, w_gate, w_up, w_down, norm_final_g,
           _cfg=None):
    x = np.asarray(x, np.float32)
    B, S, D = x.shape
    c = _cfg or Cfg(D=D, S=S, B=B)
    w_in = np.asarray(w_in, np.float32)[0]
    w_pool = np.asarray(w_pool, np.float32)[0]
    pool_scale = np.asarray(pool_scale, np.float32)[0]
    w_out = np.asarray(w_out, np.float32)[0]
    w_gate = np.asarray(w_gate, np.float32)[0]
    w_up = np.asarray(w_up, np.float32)[0]
    w_down = np.asarray(w_down, np.float32)[0]
    gmix = np.asarray(norm_mix_g, np.float32)[0]
    gffn = np.asarray(norm_ffn_g, np.float32)[0]
    gfin = np.asarray(norm_final_g, np.float32)

    key = (D, S, B)
    if key not in _PROG_CACHE:
        _PROG_CACHE[key] = build_program(c)
    nc = _PROG_CACHE[key]

    o_q, o_k, o_v, o_iq, o_ik, o_iw, o_u = 0, 2048, 2560, 3072, 4096, 4160, 4176
    shared = {}
    shared["gmix"] = vec_pk(gmix)
    shared["gffn"] = vec_pk(gffn)
    shared["gfin"] = vec_pk(gfin)
    shared["wk"] = pkc(w_in[:, o_k:o_k + 512])
    shared["wv"] = pkc(w_in[:, o_v:o_v + 512])
    wik = w_in[:, o_ik:o_ik + 64]
    shared["wik"] = pkc(np.concatenate([wik, wik], axis=1))
    shared["wiw"] = pkc(w_in[:, o_iw:o_iw + 16])
    win_cols = np.concatenate([w_in[:, o_q:o_q + 2048], w_in[:, o_iq:o_iq + 1024], w_in[:, o_u:o_u + c.POOLW]], axis=1)
    shared["win"] = slab(win_cols)
    shared["wpool"] = np.ascontiguousarray(
        w_pool.reshape(4, c.PGC, 128, c.PG).transpose(2, 0, 1, 3)).reshape(128, 4 * c.PGC * c.PG)
    shared["pscale"] = vec_pk(pool_scale)
    shared["wout"] = slab(w_out)
    shared["wgate"] = slab(w_gate)
    shared["wup"] = slab(w_up)
    shared["wdown"] = slab(w_down)
    posA = np.arange(S)
    Ca, Sa = rope_table(posA, 32, 128, 1)
    Ci, Si = rope_table(posA, 16, 64, 2)
    shared["ropeA"] = np.stack([Ca, Sa, Ci, Si]).astype(np.float32)
    cbf = np.concatenate([np.eye(128, dtype=np.float32), perm_T(32, 128, 1), perm_T(16, 64, 2),
                          np.ones((128, 128), np.float32)], axis=1)
    shared["cbf"] = cbf.astype(ml_dtypes.bfloat16)
    cf32 = np.zeros((128, 64), np.float32)
    cf32[:, 0] = EPS
    cf32[:, 1] = 1.0
    cf32[:, 8:8 + NBIS + 1] = (0.5 ** np.arange(1, NBIS + 2, dtype=np.float64)).astype(np.float32)[None, :]
    shared["cf32"] = cf32

    xT = [np.ascontiguousarray(x[b].T) for b in range(B)]
    in_maps = []
    tokmaps = []
    ncores = 4 * B
    for core in range(ncores):
        b, j = core // 4, core % 4
        m = dict(shared)
        m["xa"] = xT[b]
        xo = np.zeros((D, c.NOWN * 576), np.float32)
        pos_own = np.zeros((c.NOWN * 512,), np.int64)
        for i in range(c.NOWN):
            for r in range(4):
                blk = 16 * i + 4 * r + j
                s0 = blk * 128
                col = i * 576 + r * 144
                if s0 >= 16:
                    xo[:, col:col + 144] = xT[b][:, s0 - 16:s0 + 128]
                else:
                    xo[:, col + 16:col + 144] = xT[b][:, s0:s0 + 128]
                pos_own[i * 512 + r * 128:i * 512 + (r + 1) * 128] = np.arange(s0, s0 + 128)
        m["xo"] = xo
        CaB, SaB = rope_table(pos_own, 32, 128, 1)
        CiB, SiB = rope_table(pos_own, 16, 64, 2)
        m["ropeB"] = np.stack([CaB, SaB, CiB, SiB]).astype(np.float32)
        inv = np.zeros((c.NOWN, 4, 512), np.float32)
        for g_, w in enumerate((2, 4, 8, 16)):
            cnt = np.minimum(pos_own + 1, w).astype(np.float32)
            inv[:, g_, :] = (1.0 / cnt).reshape(c.NOWN, 512)
        m["invcnt"] = np.ascontiguousarray(np.broadcast_to(inv.reshape(1, -1), (128, c.NOWN * 4 * 512)))
        ql = np.arange(128)[:, None]
        kl = np.arange(512)[None, :]
        m["cbias"] = np.where(kl <= 128 * j + ql, 0.0, NEG).astype(np.float32)
        in_maps.append(m)
        tokmaps.append(pos_own)

    res = run_bass_kernel_spmd(nc, in_maps, core_ids=list(range(ncores)))
    if getattr(c, "debug", False):
        global _DBG
        _DBG = [np.asarray(res.results[core]["dbg"]) for core in range(ncores)]
    out = np.empty((B, S, D), np.float32)
    for core in range(ncores):
        b = core // 4
        o = np.asarray(res.results[core]["out"])
        out[b, tokmaps[core], :] = o.T
    return out
```

```python
import numpy as np
import ml_dtypes
import concourse.bass as bass
import concourse.mybir as mybir
from concourse.bass_utils import run_bass_kernel_spmd

F32 = mybir.dt.float32
BF16 = mybir.dt.bfloat16
AF = mybir.ActivationFunctionType
ALU = mybir.AluOpType
AX = mybir.AxisListType

ROPE_THETA = 500000.0
EPS = 1e-6
NBIS = 22
NEG = -1.0e30


class Cfg:
    def __init__(self, D=4096, S=8192, B=2):
        self.D, self.S, self.B = D, S, B
        self.DFF = -(-8 * D // (3 * 256)) * 256
        self.KC = D // 128
        self.FC = self.DFF // 128
        self.POOLW = D // 2
        self.PG = self.POOLW // 4
        self.PGC = self.PG // 128
        self.PC = self.POOLW // 128
        self.MIXC = 16 + self.PC
        self.NT = S // 512
        self.NOWN = S // 2048
        self.TOPK = min(256, S // 4)
        self.NE = 24 + self.PC
        self.INW = 2048 + 512 + 512 + 1024 + 64 + 16 + self.POOLW
        base, rem = divmod(self.FC, 4)
        self.QF = [base + (1 if i < rem else 0) for i in range(4)]
        self.QF = [q for q in self.QF if q > 0]


class T:
    def __init__(self, name, ap, lo=None, hi=None):
        self.name, self.ap, self.lo, self.hi = name, ap, lo, hi
        self.lastw = None
        self.rd = {}
        self.rd_dma = []
        self.ov = []
        self.chan = None
        self.ghost = False
        self.persist = False
        self.phase = None

    def __getitem__(self, idx):
        return self.ap[idx]


class Op:
    __slots__ = ("eng", "fn", "deps", "sig", "val", "sem", "dma", "waits")

    def __init__(self, eng, fn, dma):
        self.eng, self.fn, self.dma = eng, fn, dma
        self.deps = []
        self.sig = False
        self.val = None
        self.sem = None


class Sched:
    ENGS = ("pe", "act", "dve", "pool", "sp")

    def __init__(self, nc, sems):
        self.nc = nc
        self.free_sems = list(sems)
        self.esem = {e: self.free_sems.pop() for e in self.ENGS}
        self.ecount = {e: 0 for e in self.ENGS}
        self.q = {e: [] for e in self.ENGS}
        self.known = {e: {} for e in self.ENGS}
        self.chans = {}
        self.tiles = []
        self.sb_tiles = []
        self.nblk = 0
        self.phase = "init"
        self.persist_mode = True

    def begin_phase(self, tag):
        self.phase = tag
        for t in self.sb_tiles:
            if not t.persist and not t.ghost and t.phase != tag:
                t.ghost = True

    def chan(self, key):
        if key not in self.chans:
            self.chans[key] = [self.free_sems.pop(), 0]
        return self.chans[key]

    def reg_tile(self, t, sbuf=False):
        self.tiles.append(t)
        t.phase = self.phase
        t.persist = self.persist_mode
        if sbuf:
            for o in self.sb_tiles:
                if o.lo < t.hi and t.lo < o.hi:
                    o.ov.append(t)
                    t.ov.append(o)
            self.sb_tiles.append(t)
        return t

    def op(self, eng, fn, reads=(), writes=(), dma=None):
        o = Op(eng, fn, dma)
        deps = []
        is_dma = dma is not None
        rset = []
        for t in reads:
            assert not t.ghost, f"read of retired tile {t.name}"
            rset.append(t)
        wset = []
        wsrc = []
        for t in writes:
            assert not t.ghost, f"write of retired tile {t.name}"
            wset.append(t)
            wsrc.append(t)
            for a in t.ov:
                wsrc.append(a)
                if not a.ghost:
                    wset.append(a)
        for t in rset:
            for tt in [t] + t.ov:
                if tt.ghost:
                    continue
                w = tt.lastw
                if w is not None:
                    deps.append(w)
        for t in wsrc:
            w = t.lastw
            if w is not None and (is_dma or w.dma is not None or w.eng != eng):
                if not (is_dma and w.dma == dma):
                    deps.append(w)
            for e2, r in t.rd.items():
                if is_dma or e2 != eng:
                    deps.append(r)
            deps.extend(t.rd_dma)
        o.deps = [d for d in deps if d is not o]
        for d in o.deps:
            d.sig = True
        if is_dma:
            ch = self.chan(dma)
            ch[1] += 1
            o.sem, o.val = ch[0], 16 * ch[1]
            o.sig = True
        for t in rset:
            if is_dma:
                t.rd_dma.append(o)
            else:
                t.rd[eng] = o
        for t in wset:
            t.lastw = o
            t.rd = {}
            t.rd_dma = []
        self.q[eng].append(o)
        return o

    def flush(self, final_wait_all=True):
        nc = self.nc
        for e in self.ENGS:
            for o in self.q[e]:
                if o.dma is None and o.sig:
                    self.ecount[e] += 1
                    o.sem, o.val = self.esem[e], self.ecount[e]
        for e in self.ENGS:
            kn = self.known[e]
            for o in self.q[e]:
                need = {}
                for d in o.deps:
                    if d.val is None:
                        continue
                    k = id(d.sem)
                    if k not in need or need[k][1] < d.val:
                        need[k] = (d.sem, d.val)
                ws = []
                for k, (s, v) in need.items():
                    if kn.get(k, 0) >= v:
                        continue
                    kn[k] = v
                    ws.append((s, v))
                o.waits = ws
        finals = []
        if final_wait_all:
            for key, (s, c) in self.chans.items():
                if c > 0:
                    finals.append((s, 16 * c))
            for e in self.ENGS:
                if self.ecount[e] > 0:
                    finals.append((self.esem[e], self.ecount[e]))
        qs = self.q
        esem = self.esem

        def replay(ename, eng, extra=()):
            for o in qs[ename]:
                for (s, v) in o.waits:
                    eng.wait_ge(s, v)
                ins = o.fn(eng)
                if o.dma is not None:
                    ins.then_inc(o.sem, 16)
                elif o.sig:
                    ins.then_inc(o.sem, 1)
            for (s, v) in extra:
                eng.wait_ge(s, v)

        with nc.Block() as block:
            @block.tensor
            def _(eng):
                replay("pe", eng)

            @block.scalar
            def _(eng):
                replay("act", eng)

            @block.vector
            def _(eng):
                replay("dve", eng)

            @block.gpsimd
            def _(eng):
                replay("pool", eng)

            @block.sync
            def _(eng):
                replay("sp", eng, finals)
        self.q = {e: [] for e in self.ENGS}
        for t in self.tiles:
            t.lastw = None
            t.rd = {}
            t.rd_dma = []
        for t in self.sb_tiles:
            if not t.persist:
                t.ghost = True


class Arena:
    def __init__(self, sched, handle, nbytes):
        self.s, self.h, self.n = sched, handle, nbytes
        self.top = 0

    def mark(self):
        return self.top

    def at(self, off):
        self.top = off

    def release(self, m):
        self.top = m

    def alloc(self, name, shape_free, dt):
        esz = 4 if dt == F32 else 2
        n = int(np.prod(shape_free))
        nb = (n * esz + 31) // 32 * 32
        lo = self.top
        assert lo + nb <= self.n, f"SBUF arena overflow allocating {name}: {lo}+{nb} > {self.n}"
        self.top = lo + nb
        ap = self.h[:, lo // 2:(lo + n * esz) // 2]
        if dt == F32:
            ap = ap.bitcast(F32)
        if len(shape_free) == 2:
            ap = ap.rearrange("p (a b) -> p a b", b=shape_free[1])
        elif len(shape_free) == 3:
            ap = ap.rearrange("p (a b c) -> p a b c", b=shape_free[1], c=shape_free[2])
        t = T(name, ap, lo, lo + nb)
        return self.s.reg_tile(t, sbuf=True)


class Ring:
    def __init__(self, tiles):
        self.tiles = tiles
        self.i = 0

    def next(self):
        t = self.tiles[self.i % len(self.tiles)]
        self.i += 1
        return t


def build_program(cfg):
    c = cfg
    KC, S, D = c.KC, c.S, c.D
    nc = bass.Bass("TRN2", target_bir_lowering=False)

    def din(name, shape, dt=F32):
        return nc.dram_tensor(name, list(shape), dt, kind="ExternalInput")

    d_xa = din("xa", [D, S])
    d_xo = din("xo", [D, c.NOWN * 576])
    d_gmix = din("gmix", [128, KC])
    d_gffn = din("gffn", [128, KC])
    d_gfin = din("gfin", [128, KC])
    d_wk = din("wk", [128, KC, 512])
    d_wv = din("wv", [128, KC, 512])
    d_wik = din("wik", [128, KC, 128])
    d_wiw = din("wiw", [128, KC, 16])
    d_win = din("win", [c.NE, 128, KC, 128])
    d_wpool = din("wpool", [128, 4 * c.PGC * c.PG])
    d_pscale = din("pscale", [128, c.PC])
    d_wout = din("wout", [KC, 128, c.MIXC, 128])
    d_wgate = din("wgate", [c.FC, 128, KC, 128])
    d_wup = din("wup", [c.FC, 128, KC, 128])
    d_wdown = din("wdown", [KC, 128, c.FC, 128])
    d_ropeA = din("ropeA", [4, 128, S])
    d_ropeB = din("ropeB", [4, 128, c.NOWN * 512])
    d_invcnt = din("invcnt", [128, c.NOWN * 4 * 512])
    d_cbias = din("cbias", [128, 512])
    d_cbf = din("cbf", [128, 512], BF16)
    d_cf32 = din("cf32", [128, 64])
    d_out = nc.dram_tensor("out", [D, c.NOWN * 512], F32, kind="ExternalOutput")
    d_dbg = nc.dram_tensor("dbg", [128, 16384], F32, kind="ExternalOutput") if getattr(c, "debug", False) else None
    d_kc = nc.dram_tensor("kcache", [4, 128, S], BF16, kind="Internal")
    d_vc = nc.dram_tensor("vcache", [S, 512], BF16, kind="Internal")
    d_ikc = nc.dram_tensor("ikcache", [128, S], BF16, kind="Internal")

    ARENA_BYTES = 196 * 1024
    import contextlib
    with contextlib.ExitStack() as es:
        arena_h = es.enter_context(nc.sbuf_tensor("arena", [128, ARENA_BYTES // 2], BF16))
        psum = []
        for i in range(8):
            psum.append(es.enter_context(nc.psum_tensor(f"ps{i}", [128, 512], F32)))
        sems = [es.enter_context(nc.semaphore(f"s{i}")) for i in range(96)]
        sch = Sched(nc, sems)
        ar = Arena(sch, arena_h, ARENA_BYTES)
        P = [sch.reg_tile(T(f"P{i}", psum[i][:])) for i in range(8)]
        P6b = psum[6].bitcast(BF16)
        T_kc = sch.reg_tile(T("kcache", None))
        T_vc = sch.reg_tile(T("vcache", None))
        T_ikc = sch.reg_tile(T("ikcache", None))
        T_out = sch.reg_tile(T("out", None))

        cbf = ar.alloc("cbf", (512,), BF16)
        cf32 = ar.alloc("cf32", (64,), F32)
        gmix = ar.alloc("gmix", (KC,), F32)
        gffn = ar.alloc("gffn", (KC,), F32)
        gfin = ar.alloc("gfin", (KC,), F32)
        ident = cbf.ap[:, 0:128]
        PaT = cbf.ap[:, 128:256]
        PiT = cbf.ap[:, 256:384]
        ones_bf = cbf.ap[:, 384:512]
        eps_ap = cf32.ap[:, 0:1]
        one_f = cf32.ap[:, 1:2]
        pow2 = cf32.ap[:, 8:8 + NBIS + 1]

        def load(eng, t, dst_ap, src_ap, chan, reads=()):
            sch.op(eng, lambda e, o=dst_ap, i=src_ap: e.dma_start(out=o, in_=i), reads=reads, writes=[t], dma=chan)

        load("sp", cbf, cbf.ap, d_cbf.ap(), "c_cbf")
        load("sp", cf32, cf32.ap, d_cf32.ap(), "c_cf32")
        load("sp", gmix, gmix.ap, d_gmix.ap(), "c_gmix")
        load("sp", gffn, gffn.ap, d_gffn.ap(), "c_gffn")
        load("sp", gfin, gfin.ap, d_gfin.ap(), "c_gfin")
        base_mark = ar.mark()

        T_dbg = sch.reg_tile(T("dbg", None))

        def dbg_dump(t, ap, col, n):
            if d_dbg is None:
                return
            sch.op("pool", lambda e: e.dma_start(out=d_dbg.ap()[:, col:col + n], in_=ap), reads=[t], writes=[T_dbg], dma="ch_dbg")

        def mm(out_t, out_ap, lhsT, rhs, start, stop, reads):
            sch.op("pe", lambda e: e.matmul(out_ap, lhsT, rhs, start=start, stop=stop), reads=reads, writes=[out_t])

        def rope(zf_t, zf_ap, PT, Ct, C_ap, St, S_ap, out_t, out_ap, tmp1, tmp2, n):
            sch.op("pe", lambda e: e.matmul(P[3].ap[:, 0:n], PT, zf_ap, start=True, stop=True),
                   reads=[zf_t, cbf], writes=[P[3]])
            sch.op("dve", lambda e: e.tensor_tensor(tmp1.ap[:, 0:n], zf_ap, C_ap, ALU.mult),
                   reads=[zf_t, Ct], writes=[tmp1])
            sch.op("dve", lambda e: e.tensor_tensor(tmp2.ap[:, 0:n], P[3].ap[:, 0:n], S_ap, ALU.mult),
                   reads=[P[3], St], writes=[tmp2])
            sch.op("dve", lambda e: e.tensor_tensor(out_ap, tmp1.ap[:, 0:n], tmp2.ap[:, 0:n], ALU.add),
                   reads=[tmp1, tmp2], writes=[out_t])

        def rstd_from(ps_t, ps_ap, tmp_t, tmp_ap, out_t, out_ap):
            sch.op("act", lambda e: e.activation(tmp_ap, ps_ap, AF.Sqrt, bias=eps_ap, scale=1.0 / D),
                   reads=[ps_t, cf32], writes=[tmp_t])
            sch.op("dve", lambda e: e.reciprocal(out_ap, tmp_ap), reads=[tmp_t], writes=[out_t])

        assert base_mark <= 12 * 1024
        ar.at(12 * 1024)
        sch.persist_mode = False
        sch.begin_phase("A")
        wk = ar.alloc("wk", (KC, 512), BF16)
        wv = ar.alloc("wv", (KC, 512), BF16)
        wik = ar.alloc("wik", (KC, 128), BF16)
        load("pool", wk, wk.ap, d_wk.ap(), "c_wk")
        load("pool", wv, wv.ap, d_wv.ap(), "c_wv")
        load("pool", wik, wik.ap, d_wik.ap(), "c_wik")
        XG = 4
        NG = KC // XG
        xg_ring = Ring([ar.alloc(f"xg{i}", (XG, 512), F32) for i in range(2)])
        sq_ring = Ring([ar.alloc(f"sq{i}", (512,), BF16) for i in range(3)])
        hT = [[ar.alloc(f"hT{b}_{k}", (512,), BF16) for k in range(KC)] for b in range(2)]
        ropeA_t = ar.alloc("ropeA", (4, 512), F32)
        rsA = [ar.alloc(f"rsA{b}", (512,), F32) for b in range(2)]
        rsq = ar.alloc("rsq", (512,), F32)
        rtok = [ar.alloc(f"rtok{b}", (4,), F32) for b in range(2)]
        zf_ring = Ring([ar.alloc(f"zf{i}", (512,), BF16) for i in range(2)])
        tmp1 = ar.alloc("tmp1", (512,), F32)
        tmp2 = ar.alloc("tmp2", (512,), F32)
        ko_ring = Ring([ar.alloc(f"ko{i}", (512,), BF16) for i in range(3)])
        vo_ring = Ring([ar.alloc(f"vo{i}", (512,), BF16) for i in range(3)])
        pA_ring = Ring([P[1], P[2], P[4], P[5]])
        ssumA = [P[0], P[7]]

        def xstream_groups_A(t):
            b = t % 2
            groups = []
            for g in range(NG):
                def emit(g=g):
                    xg = xg_ring.next()
                    sch.op("sp", lambda e: e.dma_start(
                        out=xg.ap, in_=d_xa.ap()[g * XG * 128:(g + 1) * XG * 128, t * 512:(t + 1) * 512]
                        .rearrange("(k p) n -> p k n", p=128)), writes=[xg], dma="ch_" + xg.name)
                    for kk in range(XG):
                        kc = g * XG + kk
                        sq = sq_ring.next()
                        sch.op("act", lambda e, kk=kk, sq=sq: e.activation(sq.ap, xg.ap[:, kk, :], AF.Square),
                               reads=[xg], writes=[sq])
                        h = hT[b][kc]
                        sch.op("dve", lambda e, kk=kk, h=h, kc=kc: e.tensor_scalar(
                            h.ap, xg.ap[:, kk, :], gmix.ap[:, kc:kc + 1], None, ALU.mult),
                            reads=[xg, gmix], writes=[h])
                        mm(ssumA[b], ssumA[b].ap, ones_bf, sq.ap, kc == 0, kc == KC - 1, [sq, cbf])
                groups.append(emit)
            return groups

        def proj_groups_A(t):
            b = t % 2
            groups = []

            def g_rstd():
                sch.op("sp", lambda e: e.dma_start(
                    out=ropeA_t.ap, in_=d_ropeA.ap()[:, :, t * 512:(t + 1) * 512].rearrange("f p n -> p f n")),
                    writes=[ropeA_t], dma="ch_ropeA")
                rstd_from(ssumA[b], ssumA[b].ap, rsq, rsq.ap, rsA[b], rsA[b].ap)
                for tb in range(4):
                    sch.op("pe", lambda e, tb=tb: e.matmul(P[6].ap[:, tb:tb + 1], rsA[b].ap[0:1, tb * 128:(tb + 1) * 128],
                                                          one_f[0:1, :], start=True, stop=True),
                           reads=[rsA[b], cf32], writes=[P[6]])
                sch.op("act", lambda e: e.activation(rtok[b].ap, P[6].ap[:, 0:4], AF.Copy),
                       reads=[P[6]], writes=[rtok[b]])
            groups.append(g_rstd)

            def g_feat(ci):
                def emit():
                    ps = pA_ring.next()
                    for kc in range(KC):
                        lhsT = wk.ap[:, kc, ci * 128:(ci + 1) * 128] if ci < 4 else wik.ap[:, kc, :]
                        mm(ps, ps.ap, lhsT, hT[b][kc].ap, kc == 0, kc == KC - 1, [hT[b][kc], wk if ci < 4 else wik])
                    zf = zf_ring.next()
                    sch.op("dve", lambda e: e.tensor_tensor(zf.ap, ps.ap, rsA[b].ap, ALU.mult),
                           reads=[ps, rsA[b]], writes=[zf])
                    ko = ko_ring.next()
                    ti = 0 if ci < 4 else 2
                    rope(zf, zf.ap, PaT if ci < 4 else PiT, ropeA_t, ropeA_t.ap[:, ti, :], ropeA_t,
                         ropeA_t.ap[:, ti + 1, :], ko, ko.ap, tmp1, tmp2, 512)
                    if ci < 4:
                        sch.op("sp", lambda e: e.dma_start(out=d_kc.ap()[ci, :, t * 512:(t + 1) * 512], in_=ko.ap),
                               reads=[ko], writes=[T_kc], dma="ch_" + ko.name)
                    else:
                        sch.op("sp", lambda e: e.dma_start(out=d_ikc.ap()[:, t * 512:(t + 1) * 512], in_=ko.ap),
                               reads=[ko], writes=[T_ikc], dma="ch_" + ko.name)
                return emit
            for ci in range(5):
                groups.append(g_feat(ci))

            def g_v(tb):
                def emit():
                    ps = pA_ring.next()
                    for kc in range(KC):
                        mm(ps, ps.ap, hT[b][kc].ap[:, tb * 128:(tb + 1) * 128], wv.ap[:, kc, :], kc == 0, kc == KC - 1,
                           [hT[b][kc], wv])
                    vo = vo_ring.next()
                    sch.op("act", lambda e: e.activation(vo.ap, ps.ap, AF.Copy, scale=rtok[b].ap[:, tb:tb + 1]),
                           reads=[ps, rtok[b]], writes=[vo])
                    sch.op("sp", lambda e: e.dma_start(
                        out=d_vc.ap()[t * 512 + tb * 128: t * 512 + (tb + 1) * 128, :], in_=vo.ap),
                        reads=[vo], writes=[T_vc], dma="ch_" + vo.name)
                return emit
            for tb in range(4):
                groups.append(g_v(tb))
            return groups

        for g in xstream_groups_A(0):
            g()
        for t in range(c.NT):
            pg = proj_groups_A(t)
            xg = xstream_groups_A(t + 1) if t + 1 < c.NT else []
            n = max(len(pg), len(xg))
            for i in range(n):
                if i < len(pg):
                    pg[i]()
                if i < len(xg):
                    xg[i]()
        sch.flush()
        ar.release(base_mark)

        NQ = 576
        KB = 1024
        sch.persist_mode = True
        sch.begin_phase("Bpersist")
        ar.at(base_mark)
        wiw = ar.alloc("wiw", (KC, 16), BF16)
        pscale = ar.alloc("pscale", (c.PC,), F32)
        cbias = ar.alloc("cbias", (512,), F32)
        iw_sb = ar.alloc("iw_sb", (4, 16), F32)
        rs2 = ar.alloc("rs2", (512,), F32)
        assert ar.top <= 12 * KB, ar.top
        load("pool", wiw, wiw.ap, d_wiw.ap(), "c_wiw")
        load("sp", pscale, pscale.ap, d_pscale.ap(), "c_pscale")
        load("sp", cbias, cbias.ap, d_cbias.ap(), "c_cbias")
        WS = max(KC, c.MIXC, max(c.QF))
        ar.at(12 * KB)
        wring = Ring([ar.alloc(f"wr{i}", (WS, 128), BF16) for i in range(4)])
        mixT = ar.alloc("mixT", (c.MIXC, 512), BF16)
        OFF_H = ar.top
        hTB = [ar.alloc(f"hTB{k}", (NQ,), BF16) for k in range(KC)]
        OFF_Q = ar.top
        qT = ar.alloc("qT", (16, 512), BF16)
        iqT = ar.alloc("iqT", (8, 512), BF16)
        OFF_R = ar.top
        rsB = ar.alloc("rsB", (NQ,), F32)
        rsqB = ar.alloc("rsqB", (NQ,), F32)
        OFF_R2 = ar.top
        OFF_MIX = mixT.lo
        sch.persist_mode = False

        wstream = []

        def ws_add(ap, nk):
            wstream.append((ap, nk))
        for i in range(c.NOWN):
            for e_ in range(c.NE):
                ws_add(d_win.ap()[e_], KC)
            for dc in range(KC):
                ws_add(d_wout.ap()[dc], c.MIXC)
            f0 = 0
            for qn in c.QF:
                for f in range(f0, f0 + qn):
                    ws_add(d_wgate.ap()[f], KC)
                    ws_add(d_wup.ap()[f], KC)
                for dc in range(KC):
                    ws_add(d_wdown.ap()[dc, :, f0:f0 + qn, :], qn)
                f0 += qn
        ws_state = {"issued": 0, "consumed": 0}
        ws_slots = {}

        def ws_issue_upto(n):
            while ws_state["issued"] < min(n, len(wstream)):
                k = ws_state["issued"]
                ap, nk = wstream[k]
                slot = wring.next()
                ws_slots[k] = slot
                sch.op("pool", lambda e, slot=slot, ap=ap, nk=nk: e.dma_start(out=slot.ap[:, 0:nk, :], in_=ap),
                       writes=[slot], dma="ch_" + slot.name)
                ws_state["issued"] += 1

        def ws_get():
            k = ws_state["consumed"]
            ws_issue_upto(k + 1)
            ws_state["consumed"] += 1
            return ws_slots[k], k

        def ws_prefetch():
            ws_issue_upto(ws_state["consumed"] + 4)

        pB_ring = Ring([P[1], P[2], P[4], P[5]])

        for i in range(c.NOWN):
            sch.begin_phase(f"B2_{i}")
            ar.at(OFF_R2)
            XGB = 2
            NGB = KC // XGB
            xgB = Ring([ar.alloc(f"xgB{u}", (XGB, NQ), F32) for u in range(2)])
            sqB = Ring([ar.alloc(f"sqB{u}", (NQ,), BF16) for u in range(3)])
            tabm = ar.mark()
            ropeB_t = ar.alloc("ropeB", (4, 512), F32)
            ar.at(tabm)
            invc = ar.alloc("invc", (4, 512), F32)
            wpool = Ring([ar.alloc(f"wpool{u}", (c.PGC, c.PG), BF16) for u in range(2)])
            zfB = Ring([ar.alloc(f"zfB{u}", (512,), BF16) for u in range(2)])
            t1B = ar.alloc("t1B", (NQ,), F32)
            t2B = ar.alloc("t2B", (NQ,), F32)
            uB = Ring([ar.alloc(f"uB{u}", (NQ,), F32) for u in range(2)])
            sA = ar.alloc("sA", (NQ,), F32)
            sBt = ar.alloc("sBt", (NQ,), F32)
            diffR = Ring([[ar.alloc(f"diffT{u}_{v}", (512,), BF16) for v in range(c.PGC)] for u in range(2)])
            load("sp", ropeB_t, ropeB_t.ap, d_ropeB.ap()[:, :, i * 512:(i + 1) * 512].rearrange("f p n -> p f n"), "ch_ropeB")
            halves = [(0, NQ // 2), (NQ // 2, NQ)]
            for g in range(NGB):
                xg = xgB.next()
                sch.op("sp", lambda e, g=g, xg=xg, i=i: e.dma_start(
                    out=xg.ap, in_=d_xo.ap()[g * XGB * 128:(g + 1) * XGB * 128, i * NQ:(i + 1) * NQ]
                    .rearrange("(k p) n -> p k n", p=128)), writes=[xg], dma="ch_" + xg.name)
                for kk in range(XGB):
                    kc = g * XGB + kk
                    sq = sqB.next()
                    sch.op("act", lambda e, kk=kk, sq=sq, xg=xg: e.activation(sq.ap, xg.ap[:, kk, :], AF.Square),
                           reads=[xg], writes=[sq])
                    h = hTB[kc]
                    sch.op("dve", lambda e, kk=kk, h=h, kc=kc, xg=xg: e.tensor_scalar(
                        h.ap, xg.ap[:, kk, :], gmix.ap[:, kc:kc + 1], None, ALU.mult),
                        reads=[xg, gmix], writes=[h])
                    for hi_, (a, b_) in enumerate(halves):
                        pt = (P[0], P[7])[hi_]
                        mm(pt, pt.ap[:, 0:b_ - a], ones_bf, sq.ap[:, a:b_], kc == 0, kc == KC - 1, [sq, cbf])
            for hi_, (a, b_) in enumerate(halves):
                pt = (P[0], P[7])[hi_]
                rstd_from(pt, pt.ap[:, 0:b_ - a], rsqB, rsqB.ap[:, a:b_], rsB, rsB.ap[:, a:b_])

            def main_cols(ap2d):
                return ap2d.rearrange("p (r c) -> p r c", c=144)[:, :, 16:144]

            rsB_main = main_cols(rsB.ap)
            for e_ in range(c.NE):
                slot, _k = ws_get()
                if e_ < 24:
                    ps = pB_ring.next()
                    for kc in range(KC):
                        mm(ps, ps.ap.rearrange("p (r c) -> p r c", c=128), slot.ap[:, kc, :], main_cols(hTB[kc].ap),
                           kc == 0, kc == KC - 1, [hTB[kc], slot])
                    ws_prefetch()
                    zf = zfB.next()
                    sch.op("dve", lambda e, ps=ps, zf=zf: e.tensor_tensor(
                        zf.ap.rearrange("p (r c) -> p r c", c=128), ps.ap.rearrange("p (r c) -> p r c", c=128),
                        rsB_main, ALU.mult), reads=[ps, rsB], writes=[zf])
                    if e_ < 16:
                        rope(zf, zf.ap, PaT, ropeB_t, ropeB_t.ap[:, 0, :], ropeB_t, ropeB_t.ap[:, 1, :],
                             qT, qT.ap[:, e_, :], t1B, t2B, 512)
                    else:
                        rope(zf, zf.ap, PiT, ropeB_t, ropeB_t.ap[:, 2, :], ropeB_t, ropeB_t.ap[:, 3, :],
                             iqT, iqT.ap[:, e_ - 16, :], t1B, t2B, 512)
                else:
                    uc = e_ - 24
                    g_ = uc // c.PGC
                    kcl = uc % c.PGC
                    w = (2, 4, 8, 16)[g_]
                    if uc == 0:
                        load("sp", invc, invc.ap, d_invcnt.ap()[:, i * 2048:(i + 1) * 2048]
                             .rearrange("p (g n) -> p g n", g=4), "ch_invc")
                    if kcl == 0:
                        wpl = wpool.next()
                        diffT = diffR.next()
                        load("pool", wpl, wpl.ap, d_wpool.ap()[:, g_ * c.PGC * c.PG:(g_ + 1) * c.PGC * c.PG]
                             .rearrange("p (a b) -> p a b", b=c.PG), "ch_" + wpl.name)
                    pss = [pB_ring.next(), pB_ring.next()]
                    for kc in range(KC):
                        for hi_, (a, b_) in enumerate(halves):
                            mm(pss[hi_], pss[hi_].ap[:, 0:b_ - a], slot.ap[:, kc, :], hTB[kc].ap[:, a:b_],
                               kc == 0, kc == KC - 1, [hTB[kc], slot])
                    ws_prefetch()
                    u = uB.next()
                    for hi_, (a, b_) in enumerate(halves):
                        sch.op("dve", lambda e, hi_=hi_, a=a, b_=b_, u=u, pss=pss: e.tensor_tensor(
                            u.ap[:, a:b_], pss[hi_].ap[:, 0:b_ - a], rsB.ap[:, a:b_], ALU.mult),
                            reads=[pss[hi_], rsB], writes=[u])
                    cur, cur_t = u.ap, u
                    sh = 1
                    bufs = [sA, sBt]
                    bi = 0
                    while sh < w:
                        nt_ = bufs[bi % 2]
                        bi += 1
                        sch.op("dve", lambda e, cur=cur, nt_=nt_, sh=sh: e.tensor_tensor(
                            nt_.ap[:, sh:NQ], cur[:, sh:NQ], cur[:, 0:NQ - sh], ALU.add),
                            reads=[cur_t], writes=[nt_])
                        cur, cur_t = nt_.ap, nt_
                        sh *= 2
                    sch.op("dve", lambda e, cur=cur, g_=g_: e.tensor_tensor(
                        t1B.ap.rearrange("p (r c) -> p r c", c=144)[:, :, 0:128], main_cols(cur),
                        invc.ap[:, g_, :].rearrange("p (r c) -> p r c", c=128), ALU.mult),
                        reads=[cur_t, invc], writes=[t1B])
                    dt_ = diffT[kcl]
                    sch.op("dve", lambda e, u=u, dt_=dt_: e.tensor_tensor(
                        dt_.ap.rearrange("p (r c) -> p r c", c=128),
                        t1B.ap.rearrange("p (r c) -> p r c", c=144)[:, :, 0:128], main_cols(u.ap), ALU.subtract),
                        reads=[t1B, u], writes=[dt_])
                    if kcl == c.PGC - 1:
                        for dcn in range(c.PGC):
                            ps = pB_ring.next()
                            for kc in range(c.PGC):
                                mm(ps, ps.ap, wpl.ap[:, kc, dcn * 128:(dcn + 1) * 128], diffT[kc].ap,
                                   kc == 0, kc == c.PGC - 1, [diffT[kc], wpl])
                            ch = g_ * c.PGC + dcn
                            sch.op("act", lambda e, ps=ps, ch=ch: e.activation(mixT.ap[:, 16 + ch, :], ps.ap, AF.Copy,
                                                                             scale=pscale.ap[:, ch:ch + 1]),
                                   reads=[ps, pscale], writes=[mixT])
            for r in range(4):
                for kc in range(KC):
                    mm(P[6], P[6].ap[:, 16 * r:16 * r + 16], hTB[kc].ap[:, r * 144 + 16:r * 144 + 144], wiw.ap[:, kc, :],
                       kc == 0, kc == KC - 1, [hTB[kc], wiw])
            sch.op("act", lambda e: e.activation(iw_sb.ap.rearrange("p a b -> p (a b)"), P[6].ap[:, 0:64], AF.Copy),
                   reads=[P[6]], writes=[iw_sb])

            sch.begin_phase(f"B3_{i}")
            ar.at(OFF_H)
            scores = ar.alloc("scores", (S,), F32)
            diag_in_h = (OFF_Q - ar.top) >= 16 * 128 * 2
            if diag_in_h:
                diag = ar.alloc("diag", (16, 128), BF16)
            assert ar.top <= OFF_Q
            ar.at(OFF_R2)
            if not diag_in_h:
                diag = ar.alloc("diag", (16, 128), BF16)
            maskq = ar.alloc("maskq", (S,), BF16)
            maskT = [ar.alloc("maskT0", (S // 128, 128), BF16)] * 2
            ik_ring = Ring([ar.alloc(f"ikr{u}", (512,), BF16) for u in range(3)])
            R_ring = Ring([ar.alloc(f"Rr{u}", (512,), BF16) for u in range(3)])
            k_ring = Ring([ar.alloc(f"kr{u}", (512,), BF16) for u in range(3)])
            v_ring = Ring([ar.alloc(f"vr{u}", (4, 128), BF16) for u in range(3)])
            E_ring = Ring([ar.alloc(f"Er{u}", (512,), BF16) for u in range(3)])
            Pm_ring = Ring([ar.alloc(f"Pm{u}", (512,), BF16) for u in range(3)])
            rden = ar.alloc("rden", (512,), F32)
            bis = ar.alloc("bis", (4 * (NBIS + 4),), F32)
            L_ring = Ring([P[0], P[7]])
            ST_ring = Ring([P[1], P[2]])

            def indexer(r):
                m = 4 * i + r
                ns = m + 1
                n = 512 * ns
                for h in range(16):
                    sch.op("dve", lambda e, h=h: e.tensor_scalar(diag.ap[:, h, :], ident, iw_sb.ap[:, r, h:h + 1], None, ALU.mult),
                           reads=[cbf, iw_sb], writes=[diag])
                for s in range(ns):
                    ikt = ik_ring.next()
                    sch.op("sp", lambda e, s=s, ikt=ikt: e.dma_start(out=ikt.ap, in_=d_ikc.ap()[:, s * 512:(s + 1) * 512]),
                           reads=[T_ikc], writes=[ikt], dma="ch_" + ikt.name)
                    pend = []
                    for h in range(17):
                        if h < 16:
                            cch, off = h // 2, 64 * (h % 2)
                            L = L_ring.next()
                            mm(L, L.ap, iqT.ap[off:off + 64, cch, r * 128:(r + 1) * 128], ikt.ap[off:off + 64, :],
                               True, True, [iqT, ikt])
                            Rt = R_ring.next()
                            sch.op("act", lambda e, L=L, Rt=Rt: e.activation(Rt.ap, L.ap, AF.Relu), reads=[L], writes=[Rt])
                            pend.append((h, Rt))
                        if h >= 1:
                            hh, Rt2 = pend.pop(0)
                            mm(P[3], P[3].ap, diag.ap[:, hh, :], Rt2.ap, hh == 0, hh == 15, [diag, Rt2])
                    sch.op("act", lambda e, s=s: e.activation(scores.ap[:, s * 512:(s + 1) * 512], P[3].ap, AF.Copy),
                           reads=[P[3]], writes=[scores])
                lo0 = bis.ap[:, 0:1]
                hi0 = bis.ap[:, 1:2]
                rng = bis.ap[:, 2:3]
                steps = bis.ap[:, 4:4 + NBIS + 1]
                sch.op("dve", lambda e: e.tensor_reduce(hi0, scores.ap[:, 0:n], AX.X, ALU.max), reads=[scores], writes=[bis])
                sch.op("dve", lambda e: e.tensor_reduce(lo0, scores.ap[:, 0:n], AX.X, ALU.min), reads=[scores], writes=[bis])
                sch.op("dve", lambda e: e.tensor_tensor(scores.ap[:, n - 512:n], scores.ap[:, n - 512:n], cbias.ap, ALU.add),
                       reads=[scores, cbias], writes=[scores])
                sch.op("dve", lambda e: e.tensor_tensor(rng, hi0, lo0, ALU.subtract), reads=[bis], writes=[bis])
                sch.op("dve", lambda e: e.tensor_scalar(steps, pow2, rng, None, ALU.mult), reads=[bis, cf32], writes=[bis])
                base = 4 + NBIS + 1
                lo_c = [bis.ap[:, base + 0:base + 1], bis.ap[:, base + 1:base + 2]]
                t_c = [bis.ap[:, base + 2:base + 3], bis.ap[:, base + 3:base + 4]]
                cnt = bis.ap[:, base + 4:base + 5]
                Aap = bis.ap[:, base + 5:base + 6]
                sch.op("dve", lambda e: e.tensor_copy(lo_c[0], lo0), reads=[bis], writes=[bis])
                sch.op("dve", lambda e: e.tensor_tensor(t_c[0], lo0, steps[:, 0:1], ALU.add), reads=[bis], writes=[bis])
                for k in range(NBIS):
                    a, b_ = k % 2, (k + 1) % 2
                    sch.op("dve", lambda e, a=a: e.tensor_scalar(maskq.ap[:, 0:n], scores.ap[:, 0:n], t_c[a], 0.0,
                                                                 ALU.is_ge, ALU.add, accum_out=cnt),
                           reads=[scores, bis], writes=[maskq, bis])
                    sch.op("dve", lambda e, k=k: e.tensor_scalar(Aap, cnt, float(c.TOPK) - 0.5, steps[:, k:k + 1],
                                                                 ALU.is_ge, ALU.mult), reads=[bis], writes=[bis])
                    sch.op("dve", lambda e, a=a, b_=b_, k=k: e.scalar_tensor_tensor(
                        t_c[b_], Aap, steps[:, k + 1:k + 2], lo_c[a], ALU.add, ALU.add), reads=[bis], writes=[bis])
                    sch.op("dve", lambda e, a=a, b_=b_: e.tensor_tensor(lo_c[b_], Aap, lo_c[a], ALU.add),
                           reads=[bis], writes=[bis])
                lof = lo_c[NBIS % 2]
                sch.op("dve", lambda e: e.tensor_scalar(maskq.ap[:, 0:n], scores.ap[:, 0:n], lof, None, ALU.is_ge),
                       reads=[scores, bis], writes=[maskq])
                if i == 0 and r == 1:
                    dbg_dump(scores, scores.ap[:, 0:1024], 0, 1024)
                    dbg_dump(bis, bis.ap[:, 0:64], 1024, 64)
                    dbg_dump(maskq, maskq.ap[:, 0:1024], 2048, 1024)
                    dbg_dump(qT, qT.ap[:, 0, :], 4096, 512)
                    dbg_dump(iqT, iqT.ap[:, 0, :], 4608, 512)
                    dbg_dump(iw_sb, iw_sb.ap.rearrange("p a b -> p (a b)"), 5120, 64)

            def mask_transpose(r):
                m = 4 * i + r
                ns = m + 1
                mt = maskT[r % 2]
                for s in range(ns):
                    for j4 in range(4):
                        kc = 4 * s + j4
                        sch.op("pe", lambda e, kc=kc, j4=j4: e.transpose(P6b[:, j4 * 128:(j4 + 1) * 128],
                                                                        maskq.ap[:, kc * 128:(kc + 1) * 128], ident),
                               reads=[maskq, cbf], writes=[P[6]])
                    sch.op("act", lambda e, s=s, mt=mt: e.activation(mt.ap[:, 4 * s:4 * s + 4, :].rearrange("p a b -> p (a b)"),
                                                                     P6b[:, 0:512], AF.Copy), reads=[P[6]], writes=[mt])

            def attention(r):
                m = 4 * i + r
                ns = m + 1
                mt = maskT[r % 2]
                for nkv in range(4):
                    pend = []
                    for s in range(ns + 1):
                        if s < ns:
                            kt = k_ring.next()
                            vt = v_ring.next()
                            sch.op("sp", lambda e, s=s, kt=kt, nkv=nkv: e.dma_start(out=kt.ap, in_=d_kc.ap()[nkv, :, s * 512:(s + 1) * 512]),
                                   reads=[T_kc], writes=[kt], dma="ch_" + kt.name)
                            sch.op("sp", lambda e, s=s, vt=vt, nkv=nkv: e.dma_start(
                                out=vt.ap, in_=d_vc.ap()[s * 512:(s + 1) * 512, nkv * 128:(nkv + 1) * 128]
                                .rearrange("(a p) d -> p a d", p=128)), reads=[T_vc], writes=[vt], dma="ch_" + vt.name)
                        for j4 in range(4):
                            if s < ns:
                                kc = 4 * s + j4
                                ST = ST_ring.next()
                                mm(ST, ST.ap.rearrange("p (g q) -> p g q", q=128), kt.ap[:, j4 * 128:(j4 + 1) * 128],
                                   qT.ap[:, 4 * nkv:4 * nkv + 4, r * 128:(r + 1) * 128], True, True, [kt, qT])
                                Et = E_ring.next()
                                sch.op("act", lambda e, ST=ST, Et=Et: e.activation(Et.ap, ST.ap, AF.Exp, scale=128.0 ** -0.5),
                                       reads=[ST], writes=[Et])
                                Pm = Pm_ring.next()
                                sch.op("pool", lambda e, Et=Et, Pm=Pm, kc=kc: e.tensor_tensor(
                                    Pm.ap.rearrange("p (g q) -> p g q", q=128), Et.ap.rearrange("p (g q) -> p g q", q=128),
                                    mt.ap[:, kc:kc + 1, :].to_broadcast([128, 4, 128]), ALU.mult),
                                    reads=[Et, mt], writes=[Pm])
                                pend.append((kc, Pm, vt, j4))
                                if i == 0 and r == 1 and nkv == 0 and kc == 0:
                                    dbg_dump(kt, kt.ap, 7168, 512)
                                    dbg_dump(vt, vt.ap.rearrange("p a b -> p (a b)"), 7680, 512)
                                    dbg_dump(Et, Et.ap, 9216, 512)
                                    dbg_dump(Pm, Pm.ap, 9728, 512)
                                    dbg_dump(mt, mt.ap[:, 0:8, :].rearrange("p a b -> p (a b)"), 8192, 1024)
                            if len(pend) > (1 if s < ns else 0):
                                kc2, Pm2, vt2, j42 = pend.pop(0)
                                last = (kc2 == 4 * ns - 1)
                                mm(P[4], P[4].ap, vt2.ap[:, j42, :], Pm2.ap, kc2 == 0, last, [vt2, Pm2])
                                mm(P[5], P[5].ap, ones_bf, Pm2.ap, kc2 == 0, last, [cbf, Pm2])
                    while pend:
                        kc2, Pm2, vt2, j42 = pend.pop(0)
                        last = (kc2 == 4 * ns - 1)
                        mm(P[4], P[4].ap, vt2.ap[:, j42, :], Pm2.ap, kc2 == 0, last, [vt2, Pm2])
                        mm(P[5], P[5].ap, ones_bf, Pm2.ap, kc2 == 0, last, [cbf, Pm2])
                    sch.op("dve", lambda e: e.reciprocal(rden.ap, P[5].ap), reads=[P[5]], writes=[rden])
                    if i == 0 and r == 1 and nkv == 0:
                        dbg_dump(rden, rden.ap, 10240, 512)
                    sch.op("dve", lambda e, nkv=nkv: e.tensor_tensor(
                        mixT.ap[:, 4 * nkv:4 * nkv + 4, r * 128:(r + 1) * 128], P[4].ap.rearrange("p (g q) -> p g q", q=128),
                        rden.ap.rearrange("p (g q) -> p g q", q=128), ALU.mult), reads=[P[4], rden], writes=[mixT])

            indexer(0)
            mask_transpose(0)
            for r in range(4):
                if r + 1 < 4:
                    indexer(r + 1)
                attention(r)
                if r + 1 < 4:
                    mask_transpose(r + 1)
            if i == 0:
                dbg_dump(mixT, mixT.ap[:, 0, :], 6144, 512)

            sch.begin_phase(f"B4_{i}")
            ar.at(OFF_Q)
            x1T = [ar.alloc(f"x1T{g}", (XG, 512), F32) for g in range(NG)]
            sq2 = Ring([ar.alloc(f"sq2_{u}", (512,), BF16) for u in range(3)])
            sg_ring = Ring([ar.alloc(f"sg{u}", (512,), F32) for u in range(2)])
            rsq4 = ar.alloc("rsq4", (512,), F32)
            ar.at(OFF_MIX)
            aT = ar.alloc("aT", (max(c.QF), 512), BF16)
            assert aT.hi <= OFF_H
            h2T = hTB
            for g in range(NG):
                for kk in range(XG):
                    kc = g * XG + kk
                    sch.op("sp", lambda e, g=g, kk=kk, kc=kc, i=i: e.dma_start(
                        out=x1T[g].ap[:, kk, :].rearrange("p (r c) -> p r c", c=128),
                        in_=d_xo.ap()[kc * 128:(kc + 1) * 128, i * NQ:(i + 1) * NQ]
                        .rearrange("p (r c) -> p r c", c=144)[:, :, 16:144]),
                        writes=[x1T[g]], dma="ch_" + x1T[g].name)

            def x1(kc):
                return x1T[kc // XG], x1T[kc // XG].ap[:, kc % XG, :]

            def norm_accum(kc):
                xt_, xap = x1(kc)
                sq = sq2.next()
                sch.op("act", lambda e: e.activation(sq.ap, xap, AF.Square), reads=[xt_], writes=[sq])
                mm(P[0], P[0].ap, ones_bf, sq.ap, kc == 0, kc == KC - 1, [sq, cbf])

            for dc in range(KC):
                slot, _k = ws_get()
                ps = pB_ring.next()
                for kc in range(c.MIXC):
                    mm(ps, ps.ap, slot.ap[:, kc, :], mixT.ap[:, kc, :], kc == 0, kc == c.MIXC - 1, [mixT, slot])
                ws_prefetch()
                xt_, xap = x1(dc)
                sch.op("dve", lambda e, ps=ps, xap=xap: e.tensor_tensor(xap, ps.ap, xap, ALU.add), reads=[ps, xt_], writes=[xt_])
                norm_accum(dc)
            rstd_from(P[0], P[0].ap, rsq4, rsq4.ap, rs2, rs2.ap)
            for kc in range(KC):
                xt_, xap = x1(kc)
                sch.op("dve", lambda e, kc=kc, xap=xap: e.scalar_tensor_tensor(
                    h2T[kc].ap[:, 0:512], xap, gffn.ap[:, kc:kc + 1], rs2.ap, ALU.mult, ALU.mult),
                    reads=[xt_, gffn, rs2], writes=[h2T[kc]])
            f0 = 0
            for qi, qn in enumerate(c.QF):
                for fl in range(qn):
                    sg_, _k = ws_get()
                    su_, _k = ws_get()
                    pg_, pu_ = pB_ring.next(), pB_ring.next()
                    for kc in range(KC):
                        mm(pg_, pg_.ap, sg_.ap[:, kc, :], h2T[kc].ap[:, 0:512], kc == 0, kc == KC - 1, [h2T[kc], sg_])
                    for kc in range(KC):
                        mm(pu_, pu_.ap, su_.ap[:, kc, :], h2T[kc].ap[:, 0:512], kc == 0, kc == KC - 1, [h2T[kc], su_])
                    ws_prefetch()
                    sgt = sg_ring.next()
                    sch.op("act", lambda e, pg_=pg_, sgt=sgt: e.activation(sgt.ap, pg_.ap, AF.Silu), reads=[pg_], writes=[sgt])
                    sch.op("dve", lambda e, pu_=pu_, sgt=sgt, fl=fl: e.tensor_tensor(aT.ap[:, fl, :], pu_.ap, sgt.ap, ALU.mult),
                           reads=[pu_, sgt], writes=[aT])
                lastq = (qi == len(c.QF) - 1)
                for dc in range(KC):
                    slot, _k = ws_get()
                    ps = pB_ring.next()
                    for fl in range(qn):
                        mm(ps, ps.ap, slot.ap[:, fl, :], aT.ap[:, fl, :], fl == 0, fl == qn - 1, [aT, slot])
                    ws_prefetch()
                    xt_, xap = x1(dc)
                    sch.op("dve", lambda e, ps=ps, xap=xap: e.tensor_tensor(xap, ps.ap, xap, ALU.add),
                           reads=[ps, xt_], writes=[xt_])
                    if lastq:
                        norm_accum(dc)
                f0 += qn
            rstd_from(P[0], P[0].ap, rsq4, rsq4.ap, rs2, rs2.ap)
            for g in range(NG):
                for kk in range(XG):
                    kc = g * XG + kk
                    xt_, xap = x1(kc)
                    sch.op("dve", lambda e, kc=kc, xap=xap: e.scalar_tensor_tensor(
                        xap, xap, gfin.ap[:, kc:kc + 1], rs2.ap, ALU.mult, ALU.mult),
                        reads=[xt_, gfin, rs2], writes=[xt_])
                sch.op("sp", lambda e, g=g, i=i: e.dma_start(
                    out=d_out.ap()[g * XG * 128:(g + 1) * XG * 128, i * 512:(i + 1) * 512].rearrange("(k p) n -> p k n", p=128),
                    in_=x1T[g].ap), reads=[x1T[g]], writes=[T_out], dma="ch_" + x1T[g].name)
        sch.flush()
    return nc


def rope_table(pos, rot_dim, head_dim, nrep):
    half = rot_dim // 2
    inv_freq = (ROPE_THETA ** (-np.arange(0, rot_dim, 2, dtype=np.float32) / np.float32(rot_dim))).astype(np.float32)
    ang = pos.astype(np.float32)[None, :] * inv_freq[:, None]
    cos, sin = np.cos(ang).astype(np.float32), np.sin(ang).astype(np.float32)
    C = np.ones((head_dim, len(pos)), np.float32)
    Sg = np.zeros((head_dim, len(pos)), np.float32)
    C[0:half] = cos
    C[half:rot_dim] = cos
    Sg[0:half] = -sin
    Sg[half:rot_dim] = sin
    return np.tile(C, (nrep, 1)), np.tile(Sg, (nrep, 1))


def perm_T(rot_dim, head_dim, nrep):
    half = rot_dim // 2
    PT = np.zeros((128, 128), np.float32)
    for rep in range(nrep):
        o = rep * head_dim
        for m in range(half):
            PT[o + m + half, o + m] = 1.0
        for m in range(half, rot_dim):
            PT[o + m - half, o + m] = 1.0
    return PT


def slab(W, ncols_chunks=None):
    K, N = W.shape
    return np.ascontiguousarray(W.reshape(K // 128, 128, N // 128, 128).transpose(2, 1, 0, 3))


def pkc(W):
    K, N = W.shape
    return np.ascontiguousarray(W.reshape(K // 128, 128, N).transpose(1, 0, 2))


def vec_pk(v):
    return np.ascontiguousarray(v.reshape(-1, 128).T)


_PROG_CACHE = {}


def kernel(x, norm_mix_g, w_in, w_pool, pool_scale, w_out, norm_ffn_g, w_gate, w_up, w_down, norm_final_g,
           _cfg=None):
    x = np.asarray(x, np.float32)
    B, S, D = x.shape
    c = _cfg or Cfg(D=D, S=S, B=B)
    w_in = np.asarray(w_in, np.float32)[0]
    w_pool = np.asarray(w_pool, np.float32)[0]
    pool_scale = np.asarray(pool_scale, np.float32)[0]
    w_out = np.asarray(w_out, np.float32)[0]
    w_gate = np.asarray(w_gate, np.float32)[0]
    w_up = np.asarray(w_up, np.float32)[0]
    w_down = np.asarray(w_down, np.float32)[0]
    gmix = np.asarray(norm_mix_g, np.float32)[0]
    gffn = np.asarray(norm_ffn_g, np.float32)[0]
    gfin = np.asarray(norm_final_g, np.float32)

    key = (D, S, B)
    if key not in _PROG_CACHE:
        _PROG_CACHE[key] = build_program(c)
    nc = _PROG_CACHE[key]

    o_q, o_k, o_v, o_iq, o_ik, o_iw, o_u = 0, 2048, 2560, 3072, 4096, 4160, 4176
    shared = {}
    shared["gmix"] = vec_pk(gmix)
    shared["gffn"] = vec_pk(gffn)
    shared["gfin"] = vec_pk(gfin)
    shared["wk"] = pkc(w_in[:, o_k:o_k + 512])
    shared["wv"] = pkc(w_in[:, o_v:o_v + 512])
    wik = w_in[:, o_ik:o_ik + 64]
    shared["wik"] = pkc(np.concatenate([wik, wik], axis=1))
    shared["wiw"] = pkc(w_in[:, o_iw:o_iw + 16])
    win_cols = np.concatenate([w_in[:, o_q:o_q + 2048], w_in[:, o_iq:o_iq + 1024], w_in[:, o_u:o_u + c.POOLW]], axis=1)
    shared["win"] = slab(win_cols)
    shared["wpool"] = np.ascontiguousarray(
        w_pool.reshape(4, c.PGC, 128, c.PG).transpose(2, 0, 1, 3)).reshape(128, 4 * c.PGC * c.PG)
    shared["pscale"] = vec_pk(pool_scale)
    shared["wout"] = slab(w_out)
    shared["wgate"] = slab(w_gate)
    shared["wup"] = slab(w_up)
    shared["wdown"] = slab(w_down)
    posA = np.arange(S)
    Ca, Sa = rope_table(posA, 32, 128, 1)
    Ci, Si = rope_table(posA, 16, 64, 2)
    shared["ropeA"] = np.stack([Ca, Sa, Ci, Si]).astype(np.float32)
    cbf = np.concatenate([np.eye(128, dtype=np.float32), perm_T(32, 128, 1), perm_T(16, 64, 2),
                          np.ones((128, 128), np.float32)], axis=1)
    shared["cbf"] = cbf.astype(ml_dtypes.bfloat16)
    cf32 = np.zeros((128, 64), np.float32)
    cf32[:, 0] = EPS
    cf32[:, 1] = 1.0
    cf32[:, 8:8 + NBIS + 1] = (0.5 ** np.arange(1, NBIS + 2, dtype=np.float64)).astype(np.float32)[None, :]
    shared["cf32"] = cf32

    xT = [np.ascontiguousarray(x[b].T) for b in range(B)]
    in_maps = []
    tokmaps = []
    ncores = 4 * B
    for core in range(ncores):
        b, j = core // 4, core % 4
        m = dict(shared)
        m["xa"] = xT[b]
        xo = np.zeros((D, c.NOWN * 576), np.float32)
        pos_own = np.zeros((c.NOWN * 512,), np.int64)
        for i in range(c.NOWN):
            for r in range(4):
                blk = 16 * i + 4 * r + j
                s0 = blk * 128
                col = i * 576 + r * 144
                if s0 >= 16:
                    xo[:, col:col + 144] = xT[b][:, s0 - 16:s0 + 128]
                else:
                    xo[:, col + 16:col + 144] = xT[b][:, s0:s0 + 128]
                pos_own[i * 512 + r * 128:i * 512 + (r + 1) * 128] = np.arange(s0, s0 + 128)
        m["xo"] = xo
        CaB, SaB = rope_table(pos_own, 32, 128, 1)
        CiB, SiB = rope_table(pos_own, 16, 64, 2)
        m["ropeB"] = np.stack([CaB, SaB, CiB, SiB]).astype(np.float32)
        inv = np.zeros((c.NOWN, 4, 512), np.float32)
        for g_, w in enumerate((2, 4, 8, 16)):
            cnt = np.minimum(pos_own + 1, w).astype(np.float32)
            inv[:, g_, :] = (1.0 / cnt).reshape(c.NOWN, 512)
        m["invcnt"] = np.ascontiguousarray(np.broadcast_to(inv.reshape(1, -1), (128, c.NOWN * 4 * 512)))
        ql = np.arange(128)[:, None]
        kl = np.arange(512)[None, :]
        m["cbias"] = np.where(kl <= 128 * j + ql, 0.0, NEG).astype(np.float32)
        in_maps.append(m)
        tokmaps.append(pos_own)

    res = run_bass_kernel_spmd(nc, in_maps, core_ids=list(range(ncores)))
    if getattr(c, "debug", False):
        global _DBG
        _DBG = [np.asarray(res.results[core]["dbg"]) for core in range(ncores)]
    out = np.empty((B, S, D), np.float32)
    for core in range(ncores):
        b = core // 4
        o = np.asarray(res.results[core]["out"])
        out[b, tokmaps[core], :] = o.T
    return out
```
